# Optimizing a Trainium2 kernel written in Bass

```python
import math
import jax
import jax.numpy as jnp
from jax import lax
import numpy as np

D_MODEL = 1024
BATCH = 4
SEQ = 4096
DEPTH = 2

GRID_W = 64
CTX_LEN = 256
HEAD_DIM = 64
ATT_Q_HEADS = 8
ATT_KV_HEADS = 2
ATT_GROUP = ATT_Q_HEADS // ATT_KV_HEADS
ATT_W = ATT_Q_HEADS * HEAD_DIM
KV_W = ATT_KV_HEADS * HEAD_DIM
Q_BLOCK = 128
ROPE_THETA = 10000.0
HY_W = 256
HY_SHORT = 3
HY_EMB = 33
HY_BANDS = (HY_EMB - 1) // 2
HY_FO = 64
HY_DECAY_TARGET = 1e-2
HY_FAST_DECAY_PCT = 0.3
HY_SLOW_DECAY_PCT = 1.5
HY_MIN_DECAY = math.log(HY_DECAY_TARGET) / HY_SLOW_DECAY_PCT
HY_MAX_DECAY = math.log(HY_DECAY_TARGET) / HY_FAST_DECAY_PCT
NA_HEADS = 4
NA_W = NA_HEADS * HEAD_DIM
NA_WIN_ROWS = 8
NA_WIN_COLS = 16
D_MIX = ATT_W + HY_W + NA_W
D_IN = ATT_W + 2 * KV_W + 3 * HY_W + 3 * NA_W
SPLIT_POINTS = (ATT_W, ATT_W + KV_W, ATT_W + 2 * KV_W, ATT_W + 2 * KV_W + 3 * HY_W,
                ATT_W + 2 * KV_W + 3 * HY_W + NA_W, ATT_W + 2 * KV_W + 3 * HY_W + 2 * NA_W)
N_EXPERTS = 32
TOP_K = 4
D_EXPERT = D_MODEL
SWIGLU_ALPHA = 1.702
SWIGLU_LIMIT = 7.0
DEEPNORM_ALPHA = (2.0 * DEPTH) ** 0.25
DEEPNORM_BETA = (8.0 * DEPTH) ** -0.25
NORM_EPS = 1e-6

kernel_name = 'hybrid_gqa_hyena_natten_moe_dit'


def layer_norm(x, g=None, b=None):
    xf = x.astype(jnp.float32)
    mu = jnp.mean(xf, axis=-1, keepdims=True)
    var = jnp.mean(jnp.square(xf - mu), axis=-1, keepdims=True)
    y = (xf - mu) * lax.rsqrt(var + NORM_EPS)
    if g is not None:
        y = y * g + b
    return y.astype(x.dtype)


def rms_norm(x, g):
    xf = x.astype(jnp.float32)
    y = xf * lax.rsqrt(jnp.mean(jnp.square(xf), axis=-1, keepdims=True) + NORM_EPS) * g
    return y.astype(x.dtype)


def modulate(x, shift, scale):
    return layer_norm(x) * (1.0 + scale) + shift


def heads(t, n):
    return t.reshape(t.shape[0], t.shape[1], n, HEAD_DIM)


def rope_2d_tables(n_tokens):
    t = jnp.arange(n_tokens)
    row = (t // GRID_W).astype(jnp.float32)[:, None]
    col = (t % GRID_W).astype(jnp.float32)[:, None]
    axis_dim = HEAD_DIM // 2
    inv_freq = ROPE_THETA ** (-jnp.arange(0, axis_dim, 2, dtype=jnp.float32) / axis_dim)
    ang_r = row * inv_freq
    ang_c = col * inv_freq
    ang = jnp.concatenate([ang_r, ang_r, ang_c, ang_c], axis=-1)[:, None, :]
    return jnp.cos(ang), jnp.sin(ang)


def apply_rope_2d(x, cos, sin):
    xf = x.astype(jnp.float32)
    xs = xf.reshape(*x.shape[:-1], 2, 2, HEAD_DIM // 4)
    rot = jnp.stack([-xs[..., 1, :], xs[..., 0, :]], axis=-2).reshape(x.shape)
    return (xf * cos + rot * sin).astype(x.dtype)


def gqa_attention(q, k, v):
    s = jnp.einsum('blkgd,bskd->bkgls', q, k).astype(jnp.float32)
    p = jax.nn.softmax(s, axis=-1).astype(v.dtype)
    o = jnp.einsum('bkgls,bskd->blkgd', p, v)
    return o.reshape(o.shape[0], o.shape[1], -1)


def gqa_block_sweep(q, k, v):
    B, L = q.shape[:2]
    nb = L // Q_BLOCK
    qb = jnp.moveaxis(q.reshape(B, nb, Q_BLOCK, *q.shape[2:]), 1, 0)
    o = lax.map(lambda qq: gqa_attention(qq, k, v), qb)
    return jnp.moveaxis(o, 0, 1).reshape(B, L, -1)


def neighbourhood_attention(q, k, v, k_ctx, v_ctx, rpb):
    B, N, H, hd = q.shape
    rows = N // GRID_W
    wr = min(NA_WIN_ROWS, rows)
    wc = NA_WIN_COLS
    qg = jnp.moveaxis(q.reshape(B, rows, GRID_W, H, hd), 1, 0) * (hd ** -0.5)
    kg = k.reshape(B, rows, GRID_W, H, hd)
    vg = v.reshape(B, rows, GRID_W, H, hd)
    col = jnp.arange(GRID_W)
    col_idx = jnp.clip(col - wc // 2, 0, GRID_W - wc)[:, None] + jnp.arange(wc)[None, :]
    rel_col = col_idx - col[:, None] + (NA_WIN_COLS - 1)
    rpb_cols = rpb[:, :, rel_col]

    def row_block(args):
        r, qr = args
        rs = jnp.clip(r - wr // 2, 0, rows - wr)
        kw = lax.dynamic_slice_in_dim(kg, rs, wr, axis=1)[:, :, col_idx]
        vw = lax.dynamic_slice_in_dim(vg, rs, wr, axis=1)[:, :, col_idx]
        rel_row = rs + jnp.arange(wr) - r + (NA_WIN_ROWS - 1)
        bias = jnp.moveaxis(rpb_cols[:, rel_row], 1, 2).astype(jnp.float32)
        s_loc = jnp.einsum('bqhd,biqjhd->bhqij', qr, kw).astype(jnp.float32) + bias
        s_ctx = jnp.einsum('bqhd,bmhd->bhqm', qr, k_ctx).astype(jnp.float32)
        s = jnp.concatenate([s_loc.reshape(B, H, GRID_W, wr * wc), s_ctx], axis=-1)
        p = jax.nn.softmax(s, axis=-1).astype(v.dtype)
        p_loc = p[..., :wr * wc].reshape(B, H, GRID_W, wr, wc)
        return (jnp.einsum('bhqij,biqjhd->bqhd', p_loc, vw)
                + jnp.einsum('bhqm,bmhd->bqhd', p[..., wr * wc:], v_ctx))

    o = lax.map(row_block, (jnp.arange(rows), qg))
    return jnp.moveaxis(o, 0, 1).reshape(B, N, H * hd)


def short_conv(u, w, b):
    L = u.shape[1]
    pad = HY_SHORT // 2
    up = jnp.pad(u, ((0, 0), (pad, HY_SHORT - 1 - pad), (0, 0)))
    return sum(up[:, j:j + L] * w[j] for j in range(HY_SHORT)) + b


def hyena_filter(L, f_w1, f_b1, f_freq, f_w2, f_b2, f_w3, f_b3):
    f32 = jnp.float32
    t = jnp.linspace(0.0, 1.0, L, dtype=f32)[:, None]
    w = (2.0 * math.pi / L) * jnp.arange(L, dtype=f32)[:, None]
    bands = jnp.linspace(1e-4, HY_BANDS - 1, HY_BANDS, dtype=f32)[None, :]
    z = jnp.concatenate([t, jnp.cos(bands * w), -jnp.sin(bands * w)], axis=-1)
    hid = jnp.sin(f_freq[0] * (z @ f_w1 + f_b1))
    hid = jnp.sin(f_freq[1] * (hid @ f_w2 + f_b2))
    filt = (hid @ f_w3 + f_b3).astype(f32).reshape(L, 2, HY_W)
    deltas = jnp.linspace(HY_MIN_DECAY, HY_MAX_DECAY, HY_W, dtype=f32)
    filt = filt * jnp.exp(-t * jnp.abs(deltas))[:, None, :]
    h_fwd, h_bwd = filt[:, 0], filt[:, 1]
    g = jnp.concatenate([h_fwd, jnp.zeros((1, HY_W), f32), h_bwd[:0:-1]], axis=0)
    return g * lax.rsqrt(jnp.sum(g * g, axis=0, keepdims=True) + NORM_EPS)


def hyena_mixer(p, conv_w, conv_b, f_w1, f_b1, f_freq, f_w2, f_b2, f_w3, f_b3, d_skip):
    L = p.shape[1]
    x0, x1, v = jnp.split(short_conv(p, conv_w, conv_b), 3, axis=-1)
    g = hyena_filter(L, f_w1, f_b1, f_freq, f_w2, f_b2, f_w3, f_b3)
    z = (x1 * v).astype(jnp.float32)
    zf = jnp.fft.rfft(z, n=2 * L, axis=1)
    gf = jnp.fft.rfft(g, n=2 * L, axis=0)
    y = jnp.fft.irfft(zf * gf[None], n=2 * L, axis=1)[:, :L] + z * d_skip
    return (x0 * y).astype(p.dtype)


def token_mixer(h, hc, need_ctx, w_in, w_out, q_gain, k_gain, conv_w, conv_b, f_w1, f_b1, f_freq,
                f_w2, f_b2, f_w3, f_b3, d_skip, rpb, cos, sin):
    B, N, _ = h.shape
    M = hc.shape[1]
    scale = HEAD_DIM ** -0.5
    a_q, a_k, a_v, hy_p, n_q, n_k, n_v = jnp.split(h @ w_in, SPLIT_POINTS, axis=-1)
    if need_ctx:
        ca_q, ca_k, ca_v, chy_p, cn_q, cn_k, cn_v = jnp.split(hc @ w_in, SPLIT_POINTS, axis=-1)
    else:
        ca_k, ca_v = jnp.split(hc @ w_in[:, ATT_W:ATT_W + 2 * KV_W], 2, axis=-1)
        cn_k, cn_v = jnp.split(hc @ w_in[:, D_IN - 2 * NA_W:], 2, axis=-1)
    q = apply_rope_2d(rms_norm(heads(a_q, ATT_Q_HEADS), q_gain), cos, sin) * scale
    k = apply_rope_2d(rms_norm(heads(a_k, ATT_KV_HEADS), k_gain), cos, sin)
    ck = rms_norm(heads(ca_k, ATT_KV_HEADS), k_gain)
    cv = heads(ca_v, ATT_KV_HEADS)
    k_all = jnp.concatenate([k, ck], axis=1)
    v_all = jnp.concatenate([heads(a_v, ATT_KV_HEADS), cv], axis=1)
    y_att = gqa_block_sweep(q.reshape(B, N, ATT_KV_HEADS, ATT_GROUP, HEAD_DIM), k_all, v_all)
    y_hy = hyena_mixer(hy_p, conv_w, conv_b, f_w1, f_b1, f_freq, f_w2, f_b2, f_w3, f_b3, d_skip)
    nk_c = heads(cn_k, NA_HEADS)
    nv_c = heads(cn_v, NA_HEADS)
    y_na = neighbourhood_attention(heads(n_q, NA_HEADS), heads(n_k, NA_HEADS), heads(n_v, NA_HEADS),
                                   nk_c, nv_c, rpb)
    y = jnp.concatenate([y_att, y_hy, y_na], axis=-1) @ w_out
    if not need_ctx:
        return y, None
    cq = rms_norm(heads(ca_q, ATT_Q_HEADS), q_gain) * scale
    yc_att = gqa_attention(cq.reshape(B, M, ATT_KV_HEADS, ATT_GROUP, HEAD_DIM), ck, cv)
    yc_hy = hyena_mixer(chy_p, conv_w, conv_b, f_w1, f_b1, f_freq, f_w2, f_b2, f_w3, f_b3, d_skip)
    yc_na = gqa_attention(heads(cn_q, NA_HEADS)[:, :, :, None] * scale, nk_c, nv_c)
    yc = jnp.concatenate([yc_att, yc_hy, yc_na], axis=-1) @ w_out
    return y, yc


def expert_ffn(t, router_w, router_b, w1, b1, w2, b2):
    logits = (t @ router_w + router_b).astype(jnp.float32)
    top_v, top_i = lax.top_k(logits, TOP_K)
    wts = jax.nn.softmax(top_v, axis=-1)
    gates = jnp.sum(jax.nn.one_hot(top_i, N_EXPERTS, dtype=jnp.float32) * wts[..., None], axis=1)
    gates = gates.astype(t.dtype)
    out = jnp.zeros_like(t)
    for e in range(N_EXPERTS):
        hid = t @ w1[e] + b1[e]
        glu = jnp.minimum(hid[:, :D_EXPERT], SWIGLU_LIMIT)
        lin = jnp.clip(hid[:, D_EXPERT:], -SWIGLU_LIMIT, SWIGLU_LIMIT)
        act = glu * jax.nn.sigmoid(SWIGLU_ALPHA * glu) * (lin + 1.0)
        out = out + gates[:, e:e + 1] * (act @ w2[e] + b2[e])
    return out


def setup_inputs(seed: int = 0) -> dict:
    key = jax.random.key(seed)
    ks = iter(jax.random.split(key, 31))

    def nrm(shape, scale):
        return scale * jax.random.normal(next(ks), shape, jnp.float32)

    L = DEPTH
    return {
        'x': nrm((BATCH, SEQ, D_MODEL), 1.0),
        'c': nrm((BATCH, D_MODEL), 1.0),
        'ctx': nrm((BATCH, CTX_LEN, D_MODEL), 1.0),
        'c_ctx': nrm((D_MODEL,), 1.0),
        'ada_w': nrm((L, D_MODEL, 6 * D_MODEL), 0.5 * D_MODEL ** -0.5),
        'ada_b': nrm((L, 6 * D_MODEL), 0.01),
        'w_in': nrm((L, D_MODEL, D_IN), D_MODEL ** -0.5),
        'w_out': nrm((L, D_MIX, D_MODEL), DEEPNORM_BETA * D_MIX ** -0.5),
        'q_gain': 1.0 + nrm((L, HEAD_DIM), 0.01),
        'k_gain': 1.0 + nrm((L, HEAD_DIM), 0.01),
        'hy_conv_w': nrm((L, HY_SHORT, 3 * HY_W), HY_SHORT ** -0.5),
        'hy_conv_b': nrm((L, 3 * HY_W), 0.01),
        'hy_w1': nrm((L, HY_EMB, HY_FO), HY_EMB ** -0.5),
        'hy_b1': nrm((L, HY_FO), 0.01),
        'hy_freq': 1.0 + nrm((L, 2, HY_FO), 0.01),
        'hy_w2': nrm((L, HY_FO, HY_FO), HY_FO ** -0.5),
        'hy_b2': nrm((L, HY_FO), 0.01),
        'hy_w3': nrm((L, HY_FO, 2 * HY_W), HY_FO ** -0.5),
        'hy_b3': nrm((L, 2 * HY_W), 0.01),
        'hy_d': nrm((L, HY_W), 0.5),
        'na_rpb': nrm((L, NA_HEADS, 2 * NA_WIN_ROWS - 1, 2 * NA_WIN_COLS - 1), 0.05),
        'ln1_g': 1.0 + nrm((L, D_MODEL), 0.01),
        'ln1_b': nrm((L, D_MODEL), 0.01),
        'ln2_g': 1.0 + nrm((L, D_MODEL), 0.01),
        'ln2_b': nrm((L, D_MODEL), 0.01),
        'router_w': nrm((L, D_MODEL, N_EXPERTS), D_MODEL ** -0.5),
        'router_b': nrm((L, N_EXPERTS), 0.01),
        'exp_w1': nrm((L, N_EXPERTS, D_MODEL, 2 * D_EXPERT), D_MODEL ** -0.5),
        'exp_b1': nrm((L, N_EXPERTS, 2 * D_EXPERT), 0.01),
        'exp_w2': nrm((L, N_EXPERTS, D_EXPERT, D_MODEL), DEEPNORM_BETA * D_EXPERT ** -0.5),
        'exp_b2': nrm((L, N_EXPERTS, D_MODEL), 0.01),
    }


def reference(x, c, ctx, c_ctx, ada_w, ada_b, w_in, w_out, q_gain, k_gain, hy_conv_w, hy_conv_b,
              hy_w1, hy_b1, hy_freq, hy_w2, hy_b2, hy_w3, hy_b3, hy_d, na_rpb, ln1_g, ln1_b,
              ln2_g, ln2_b, router_w, router_b, exp_w1, exp_b1, exp_w2, exp_b2):
    B, N, D = x.shape
    M = ctx.shape[1]
    cos, sin = rope_2d_tables(N)
    c_act = jax.nn.silu(c)
    cc_act = jax.nn.silu(c_ctx)
    for l in range(DEPTH):
        need_ctx = l < DEPTH - 1
        mod = jnp.split((c_act @ ada_w[l] + ada_b[l])[:, None, :], 6, axis=-1)
        cmod = jnp.split(cc_act @ ada_w[l] + ada_b[l], 6, axis=-1)
        y, yc = token_mixer(modulate(x, mod[0], mod[1]), modulate(ctx, cmod[0], cmod[1]), need_ctx,
                            w_in[l], w_out[l], q_gain[l], k_gain[l], hy_conv_w[l], hy_conv_b[l],
                            hy_w1[l], hy_b1[l], hy_freq[l], hy_w2[l], hy_b2[l], hy_w3[l], hy_b3[l],
                            hy_d[l], na_rpb[l], cos, sin)
        x = layer_norm(DEEPNORM_ALPHA * x + mod[2] * y, ln1_g[l], ln1_b[l])
        if need_ctx:
            ctx = layer_norm(DEEPNORM_ALPHA * ctx + cmod[2] * yc, ln1_g[l], ln1_b[l])
            h = modulate(x, mod[3], mod[4]).reshape(B * N, D)
            hc = modulate(ctx, cmod[3], cmod[4]).reshape(B * M, D)
            out = expert_ffn(jnp.concatenate([h, hc], axis=0), router_w[l], router_b[l],
                             exp_w1[l], exp_b1[l], exp_w2[l], exp_b2[l])
            y = out[:B * N].reshape(B, N, D)
            ctx = layer_norm(DEEPNORM_ALPHA * ctx + cmod[5] * out[B * N:].reshape(B, M, D),
                             ln2_g[l], ln2_b[l])
        else:
            y = expert_ffn(modulate(x, mod[3], mod[4]).reshape(B * N, D), router_w[l], router_b[l],
                           exp_w1[l], exp_b1[l], exp_w2[l], exp_b2[l]).reshape(B, N, D)
        x = layer_norm(DEEPNORM_ALPHA * x + mod[5] * y, ln2_g[l], ln2_b[l])
    return x
```

```python
import numpy as np
from contextlib import ExitStack
import concourse.bass as bass
import concourse.mybir as mybir
from concourse.bass_utils import run_bass_kernel_spmd

F32 = mybir.dt.float32
BF16 = mybir.dt.bfloat16
AF = mybir.ActivationFunctionType
ALU = mybir.AluOpType
AX = mybir.AxisListType


class Buf:
    __slots__ = ("t", "w", "r", "name")

    def __init__(self, t, name=""):
        self.t = t
        self.w = None
        self.r = {}
        self.name = name

    def __getitem__(self, k):
        return self.t[k]


class TR:
    NDMA = 24

    def __init__(self, nc, es, tag=""):
        self.nc = nc
        self.es = es
        self.eng = {"pe": nc.tensor, "act": nc.scalar, "dve": nc.vector, "pool": nc.gpsimd, "sp": nc.sync}
        self.sem = {}
        self.cnt = {}
        for k in ("pe", "act", "dve", "pool"):
            self.sem[k] = es.enter_context(nc.semaphore(f"{tag}s_{k}"))
            self.cnt[k] = 0
        for i in range(self.NDMA):
            k = f"d{i}"
            self.sem[k] = es.enter_context(nc.semaphore(f"{tag}s_{k}"))
            self.cnt[k] = 0
        self.waited = {}
        self.dma_rr = 0
        self.pending = {k: False for k in ("pe", "act", "dve", "pool")}
        self.nbuf = 0

    def sb(self, shape, dtype=F32, name=None):
        self.nbuf += 1
        name = name or f"sb{self.nbuf}"
        t = self.es.enter_context(self.nc.sbuf_tensor(f"{name}_{self.nbuf}", list(shape), dtype))
        return Buf(t, name)

    def ps(self, shape, dtype=F32, name=None):
        self.nbuf += 1
        name = name or f"ps{self.nbuf}"
        t = self.es.enter_context(self.nc.psum_tensor(f"{name}_{self.nbuf}", list(shape), dtype))
        return Buf(t, name)

    def dram(self, name, shape, dtype=F32, kind="Internal"):
        t = self.nc.dram_tensor(name, list(shape), dtype, kind=kind)
        return Buf(t.ap(), name)

    def _deps(self, R, W):
        deps = {}
        for b in R:
            if b.w is not None:
                k, v = b.w
                deps[k] = max(deps.get(k, 0), v)
        for b in W:
            if b.w is not None:
                k, v = b.w
                deps[k] = max(deps.get(k, 0), v)
            for k, v in b.r.items():
                deps[k] = max(deps.get(k, 0), v)
        return deps

    def _wait(self, e, deps, skip_self=False):
        engine = self.eng[e]
        for k, v in deps.items():
            if skip_self and k == e:
                continue
            if self.waited.get((e, k), 0) >= v:
                continue
            engine.wait_ge(self.sem[k], v)
            self.waited[(e, k)] = v

    def _mark(self, ev, R, W):
        k, v = ev
        for b in W:
            b.w = ev
            b.r = {}
        for b in R:
            if b.r.get(k, 0) < v:
                b.r[k] = v

    def op(self, e, fn, R=(), W=(), sig=True):
        deps = self._deps(R, W)
        self._wait(e, deps, skip_self=(e == "pe"))
        ins = fn(self.eng[e])
        if sig:
            self.cnt[e] += 1
            ins.then_inc(self.sem[e], 1)
            ev = (e, self.cnt[e])
        else:
            ev = (e, self.cnt[e] + 1)
        self._mark(ev, R, W)
        return ins

    def dma(self, q, out, in_, R=(), W=(), **kw):
        deps = self._deps(R, W)
        self._wait(q, deps)
        slot = f"d{self.dma_rr}"
        self.dma_rr = (self.dma_rr + 1) % self.NDMA
        prev = self.cnt[slot]
        if prev > 0 and self.waited.get((q, slot), 0) < prev:
            self.eng[q].wait_ge(self.sem[slot], prev)
            self.waited[(q, slot)] = prev
        ins = self.eng[q].dma_start(out=out, in_=in_, **kw)
        self.cnt[slot] = prev + 16
        ins.then_inc(self.sem[slot], 16)
        self._mark((slot, prev + 16), R, W)
        return ins

    def barrier(self):
        for e in ("pe", "act", "dve", "pool", "sp"):
            self._wait(e, dict((k, v) for k, v in self.cnt.items() if v > 0))

    def finish(self):
        sp = self.nc.sync
        for k, v in self.cnt.items():
            if v > 0:
                sp.wait_ge(self.sem[k], v)


def interleave(gens, width=2):
    gens = list(gens)
    active = []
    nxt = 0
    while active or nxt < len(gens):
        while len(active) < width and nxt < len(gens):
            active.append(gens[nxt])
            nxt += 1
        for g in list(active):
            try:
                next(g)
            except StopIteration:
                active.remove(g)


D = 1024
DIN = 2304
NLAT = 4096
NCTX = 256
NTOK = NLAT + NCTX
EPS = 1e-6


def bcast_rows(ap_row, nparts=128):
    return ap_row.partition_broadcast(nparts)


def stage_mod(nc, tr, c2T, ada_w, ada_b, modrow):
    with ExitStack() as es:
        tr.es = es
        cs = tr.sb([128, 8, 2], F32, "cs")
        sc = tr.sb([128, 8, 2], F32, "sc")
        sg = tr.sb([128, 8, 2], F32, "sg")
        tr.dma("sp", cs[:], c2T[:], R=[c2T], W=[cs])
        tr.op("act", lambda e: e.activation(out=sg[:], in_=cs[:], func=AF.Sigmoid), R=[cs], W=[sg])
        tr.op("dve", lambda e: e.tensor_mul(out=sc[:], in0=cs[:], in1=sg[:]), R=[cs, sg], W=[sc])
        bias = tr.sb([2, 6144], F32, "bias")
        tr.dma("sp", bias[:], ada_b[:].partition_broadcast(2), R=[ada_b], W=[bias])
        res = tr.sb([2, 6144], F32, "res")
        wbufs = [tr.sb([128, 8, 512], F32, f"aw{i}") for i in range(2)]
        pss = [tr.ps([128, 512], F32, f"pm{i}") for i in range(2)]
        for j in range(12):
            wb = wbufs[j % 2]
            ps = pss[j % 2]
            tr.dma("sp", wb[:], ada_w[:, j * 512:(j + 1) * 512].rearrange("(k p) n -> p k n", p=128), R=[ada_w], W=[wb])
            for k in range(8):
                tr.op("pe", lambda e, k=k: e.matmul(ps[0:2, :], lhsT=sc[:, k, :], rhs=wb[:, k, :], start=(k == 0), stop=(k == 7)),
                      R=[sc, wb], W=[ps], sig=(k == 7))
            tr.op("dve", lambda e: e.tensor_add(out=res[:, j * 512:(j + 1) * 512], in0=ps[0:2, :], in1=bias[:, j * 512:(j + 1) * 512]),
                  R=[ps, bias], W=[res])
        tr.dma("sp", modrow[:], res[:], R=[res], W=[modrow])
        tr.barrier()


def load_modT(nc, tr, modrow, r, names):
    t = tr.sb([128, 48], F32, "modT")
    with nc.allow_non_contiguous_dma(reason="tiny"):
        tr.dma("sp", t[:], modrow[r, :].rearrange("(j p) -> p j", p=128), R=[modrow], W=[t])
    return t


def ln_stats(tr, xt, stats, mv, rstd, tmp):
    for hseg in range(2):
        tr.op("dve", lambda e, hseg=hseg: e.bn_stats(out=stats[:, hseg * 6:(hseg + 1) * 6], in_=xt[:, hseg * 512:(hseg + 1) * 512]),
              R=[xt], W=[stats])
    tr.op("dve", lambda e: e.bn_aggr(out=mv[:], in_=stats[:]), R=[stats], W=[mv])
    tr.op("act", lambda e: e.activation(out=tmp[:], in_=mv[:, 1:2], func=AF.Sqrt, bias=EPS), R=[mv], W=[tmp])
    tr.op("dve", lambda e: e.reciprocal(out=rstd[:], in_=tmp[:]), R=[tmp], W=[rstd])


def stage_inproj(nc, tr, xsrc, modrow, w_in, projT, vtm, ident_bf):
    with ExitStack() as es:
        tr.es = es
        w = tr.sb([128, 8, DIN], BF16, "w_in")
        for k in range(8):
            tr.dma("pool", w[:, k, :], w_in[k * 128:(k + 1) * 128, :], R=[w_in], W=[w])
        mods = []
        for r in range(2):
            mT = load_modT(nc, tr, modrow, r, None)
            sp1 = tr.sb([128, 8], F32, "sp1")
            tr.op("dve", lambda e, mT=mT, sp1=sp1: e.tensor_scalar_add(out=sp1[:], in0=mT[:, 8:16], scalar1=1.0), R=[mT], W=[sp1])
            mods.append((mT, sp1))
        NB = NTOK // 512 + (1 if NTOK % 512 else 0)
        xts = [tr.sb([128, D], F32, f"xt{i}") for i in range(3)]
        xns = [tr.sb([128, D], BF16, f"xn{i}") for i in range(2)]
        hTs = [tr.sb([128, 8, 512], BF16, f"hT{i}") for i in range(2)]
        lnb_ = [[tr.sb([128, 12], F32, f"stats{i}"), tr.sb([128, 2], F32, f"mv{i}"), tr.sb([128, 1], F32, f"rstd{i}"),
                 tr.sb([128, 1], F32, f"tmp{i}"), tr.sb([128, 1], F32, f"nmr{i}")] for i in range(3)]
        pT = [tr.ps([128, 8, 128], BF16, f"pT{i}") for i in range(2)]
        pO = [tr.ps([128, 512], F32, f"pO{i}") for i in range(4)]
        osb = [tr.sb([128, 512], F32, f"osb{i}") for i in range(4)]
        vsb = [tr.sb([128, 384], BF16, f"vsb{i}") for i in range(2)]
        st = {"it": 0, "oi": 0}

        def phaseA(blk):
            t0 = blk * 512
            nt = min(512, NTOK - t0)
            ntile = nt // 128
            hT = hTs[blk % 2]
            r = 1 if t0 >= NLAT else 0
            mT, sp1 = mods[r]
            for ti in range(ntile):
                it = st["it"]
                xt = xts[it % 3]
                xn = xns[it % 2]
                p = pT[it % 2]
                stats, mv, rstd, tmp, nmr = lnb_[it % 3]
                st["it"] += 1
                tok = t0 + ti * 128
                tr.dma("sp", xt[:], xsrc[tok:tok + 128, :], R=[xsrc], W=[xt])
                ln_stats(tr, xt, stats, mv, rstd, tmp)
                tr.op("dve", lambda e: e.scalar_tensor_tensor(out=nmr[:], in0=mv[:, 0:1], scalar=-1.0, in1=rstd[:], op0=ALU.mult, op1=ALU.mult),
                      R=[mv, rstd], W=[nmr])
                tr.op("act", lambda e, xt=xt, xn=xn: e.activation(out=xn[:], in_=xt[:], func=AF.Identity, scale=rstd[:], bias=nmr[:]),
                      R=[xt, rstd, nmr], W=[xn])
                yield
                for c in range(8):
                    tr.op("pe", lambda e, c=c, xn=xn, p=p: e.transpose(out=p[:, c, :], in_=xn[:, c * 128:(c + 1) * 128], identity=ident_bf[:]),
                          R=[xn, ident_bf], W=[p], sig=(c == 7))
                for c in range(8):
                    tr.op("dve" if c % 2 else "act",
                          (lambda e, c=c, p=p: e.tensor_scalar(out=hT[:, c, ti * 128:(ti + 1) * 128], in0=p[:, c, :], scalar1=sp1[:, c:c + 1],
                                                               scalar2=mT[:, c:c + 1], op0=ALU.mult, op1=ALU.add)) if c % 2 else
                          (lambda e, c=c, p=p: e.activation(out=hT[:, c, ti * 128:(ti + 1) * 128], in_=p[:, c, :], func=AF.Identity,
                                                            scale=sp1[:, c:c + 1], bias=mT[:, c:c + 1])),
                          R=[p, sp1, mT], W=[hT])
                yield

        def phaseB(blk):
            t0 = blk * 512
            nt = min(512, NTOK - t0)
            ntile = nt // 128
            hT = hTs[blk % 2]
            for cc in range(DIN // 128):
                oi = st["oi"]
                po = pO[oi % 4]
                ob = osb[oi % 4]
                st["oi"] += 1
                for k in range(8):
                    tr.op("pe", lambda e, k=k, cc=cc, po=po: e.matmul(po[:, 0:nt], lhsT=w[:, k, cc * 128:(cc + 1) * 128], rhs=hT[:, k, 0:nt],
                                                                      start=(k == 0), stop=(k == 7)), R=[w, hT], W=[po], sig=(k == 7))
                tr.op("dve" if cc % 2 else "act",
                      (lambda e, po=po, ob=ob: e.tensor_copy(out=ob[:, 0:nt], in_=po[:, 0:nt])) if cc % 2 else
                      (lambda e, po=po, ob=ob: e.activation(out=ob[:, 0:nt], in_=po[:, 0:nt], func=AF.Copy)),
                      R=[po], W=[ob])
                tr.dma("sp", projT[cc * 128:(cc + 1) * 128, t0:t0 + nt], ob[:, 0:nt], R=[ob], W=[projT])
                yield
            for ti in range(ntile):
                oi = st["oi"]
                po = pO[oi % 4]
                vb = vsb[oi % 2]
                st["oi"] += 1
                for k in range(8):
                    tr.op("pe", lambda e, k=k, po=po: e.matmul(po[:, 0:128], lhsT=hT[:, k, ti * 128:(ti + 1) * 128], rhs=w[:, k, 640:768],
                                                               start=(k == 0), stop=(k == 7)), R=[w, hT], W=[po], sig=False)
                for k in range(8):
                    tr.op("pe", lambda e, k=k, po=po: e.matmul(po[:, 128:384], lhsT=hT[:, k, ti * 128:(ti + 1) * 128], rhs=w[:, k, 2048:2304],
                                                               start=(k == 0), stop=(k == 7)), R=[w, hT], W=[po], sig=(k == 7))
                tr.op("dve", lambda e, po=po, vb=vb: e.tensor_copy(out=vb[:], in_=po[:, 0:384]), R=[po], W=[vb])
                tok = t0 + ti * 128
                tr.dma("sp", vtm[tok:tok + 128, :], vb[:], R=[vb], W=[vtm])
                yield

        for _ in phaseA(0):
            pass
        for blk in range(NB):
            gB = phaseB(blk)
            gA = phaseA(blk + 1) if blk + 1 < NB else None
            doneA = gA is None
            doneB = False
            while not (doneA and doneB):
                for _ in range(3):
                    if not doneB:
                        try:
                            next(gB)
                        except StopIteration:
                            doneB = True
                if not doneA:
                    try:
                        next(gA)
                    except StopIteration:
                        doneA = True
        tr.barrier()


NOWN = 2048
NQ = NOWN + NCTX
NKC = NTOK // 128


def stage_qkprep(nc, tr, projT, q_gain, k_gain, costab, sintab, blockones, perm, QT, KT, need_ctx):
    with ExitStack() as es:
        tr.es = es
        bo = tr.sb([128, 128], BF16, "bo")
        pm = tr.sb([128, 128], BF16, "pm")
        tr.dma("sp", bo[:], blockones[:], R=[blockones], W=[bo])
        tr.dma("sp", pm[:], perm[:], R=[perm], W=[pm])
        gq = tr.sb([128, 1], F32, "gq")
        gk = tr.sb([128, 1], F32, "gk")
        for g, src in ((gq, q_gain), (gk, k_gain)):
            for half in range(2):
                tr.dma("sp", g[half * 64:(half + 1) * 64, :], src[:].rearrange("(p o) -> p o", o=1), R=[src], W=[g])
        R3 = 3
        xs = [tr.sb([128, 512], F32, f"x{i}") for i in range(R3)]
        cs = [tr.sb([128, 512], F32, f"c{i}") for i in range(R3)]
        ss_ = [tr.sb([128, 512], F32, f"s{i}") for i in range(R3)]
        sq = [tr.sb([128, 512], BF16, f"sq{i}") for i in range(2)]
        sd = [tr.sb([128, 512], F32, f"sd{i}") for i in range(2)]
        ri = [tr.sb([128, 512], F32, f"ri{i}") for i in range(2)]
        yb = [tr.sb([128, 512], BF16, f"yb{i}") for i in range(2)]
        t1 = [tr.sb([128, 512], F32, f"t1{i}") for i in range(2)]
        t2 = [tr.sb([128, 512], F32, f"t2{i}") for i in range(2)]
        ob = [tr.sb([128, 512], BF16, f"ob{i}") for i in range(2)]
        pA = [tr.ps([128, 512], F32, f"pA{i}") for i in range(2)]
        pB = [tr.ps([128, 512], F32, f"pB{i}") for i in range(2)]
        work = []
        own_blocks = [(i * 512, 512) for i in range(4)]
        oth_blocks = [(NOWN + i * 512, 512) for i in range(4)]
        ctx_block = [(NLAT, NCTX)]
        for rg in range(4):
            for (t0, nt) in own_blocks + (ctx_block if need_ctx else []):
                work.append((rg * 128, QT, rg * 128, gq, t0, nt))
        for (t0, nt) in own_blocks + oth_blocks + ctx_block:
            work.append((512, KT, 0, gk, t0, nt))
        for i, (srow, dst, drow, g, t0, nt) in enumerate(work):
            x = xs[i % R3]; c = cs[i % R3]; s = ss_[i % R3]
            j = i % 2
            tr.dma("sp", x[:, 0:nt], projT[srow:srow + 128, t0:t0 + nt], R=[projT], W=[x])
            tr.dma("sp", c[:, 0:nt], costab[:, t0:t0 + nt], R=[costab], W=[c])
            tr.dma("sp", s[:, 0:nt], sintab[:, t0:t0 + nt], R=[sintab], W=[s])
            tr.op("act", lambda e: e.activation(out=sq[j][:, 0:nt], in_=x[:, 0:nt], func=AF.Square), R=[x], W=[sq[j]])
            tr.op("pe", lambda e: e.matmul(pA[j][:, 0:nt], lhsT=bo[:], rhs=sq[j][:, 0:nt], start=True, stop=True), R=[bo, sq[j]], W=[pA[j]])
            tr.op("act", lambda e: e.activation(out=sd[j][:, 0:nt], in_=pA[j][:, 0:nt], func=AF.Sqrt, scale=1.0 / 64, bias=EPS), R=[pA[j]], W=[sd[j]])
            tr.op("dve", lambda e: e.reciprocal(out=ri[j][:, 0:nt], in_=sd[j][:, 0:nt]), R=[sd[j]], W=[ri[j]])
            tr.op("dve", lambda e: e.scalar_tensor_tensor(out=yb[j][:, 0:nt], in0=x[:, 0:nt], scalar=g[:, 0:1], in1=ri[j][:, 0:nt], op0=ALU.mult, op1=ALU.mult),
                  R=[x, g, ri[j]], W=[yb[j]])
            tr.op("pe", lambda e: e.matmul(pB[j][:, 0:nt], lhsT=pm[:], rhs=yb[j][:, 0:nt], start=True, stop=True), R=[pm, yb[j]], W=[pB[j]])
            tr.op("pool", lambda e: e.tensor_mul(out=t1[j][:, 0:nt], in0=yb[j][:, 0:nt], in1=c[:, 0:nt]), R=[yb[j], c], W=[t1[j]])
            tr.op("dve", lambda e: e.tensor_mul(out=t2[j][:, 0:nt], in0=pB[j][:, 0:nt], in1=s[:, 0:nt]), R=[pB[j], s], W=[t2[j]])
            tr.op("dve", lambda e: e.tensor_add(out=ob[j][:, 0:nt], in0=t1[j][:, 0:nt], in1=t2[j][:, 0:nt]), R=[t1[j], t2[j]], W=[ob[j]])
            tr.dma("sp", dst[drow:drow + 128, t0:t0 + nt], ob[j][:, 0:nt], R=[ob[j]], W=[dst])
        tr.barrier()


class AttnCtx:
    def __init__(self, tr):
        self.pS = [tr.ps([128, 512], F32, f"pS{i}") for i in range(4)]
        self.pO = [tr.ps([64, 512], F32, f"pOa{i}") for i in range(2)]
        self.pZ = [tr.ps([64, 512], F32, f"pZ{i}") for i in range(2)]
        self.PT = [tr.sb([128, 512], BF16, f"PT{i}") for i in range(4)]
        self.rinv = [tr.sb([64, 512], F32, f"rinv{i}") for i in range(2)]
        self.yo = [tr.sb([64, 512], BF16, f"yo{i}") for i in range(2)]
        self.ones = tr.sb([128, 64], BF16, "ones")
        tr.op("dve", lambda e: e.memset(self.ones[:], 1.0), W=[self.ones])
        self.i = 0
        self.o = 0


def attn_block(tr, ac, *a, **kw):
    for _ in attn_block_gen(tr, ac, *a, **kw):
        pass


def attn_block_gen(tr, ac, Qh, q0, nq, Kg, V, vcol, chunks, YT, yrow, ycol, mask_fn=None, pre=None):
    if pre is not None:
        pre()
    o = ac.o % 2
    ac.o += 1
    pO, pZ = ac.pO[o], ac.pZ[o]
    n = len(chunks)
    slots = []

    def issue_qk(ci):
        i = ac.i % 4
        ac.i += 1
        pS, PT = ac.pS[i], ac.PT[i]
        ch = chunks[ci]
        tr.op("pe", lambda e: e.matmul(pS[:, 0:nq], lhsT=Kg[:, ch * 128:(ch + 1) * 128], rhs=Qh[:, q0:q0 + nq], start=True, stop=True),
              R=[Kg, Qh], W=[pS])
        slots.append((pS, PT))

    LOOK = 3
    for ci in range(min(LOOK, n)):
        issue_qk(ci)
    for ci, ch in enumerate(chunks):
        pS, PT = slots[ci]
        tr.op("act", lambda e: e.activation(out=PT[:, 0:nq], in_=pS[:, 0:nq], func=AF.Exp, scale=0.125), R=[pS], W=[PT])
        if mask_fn is not None:
            m = mask_fn(ci)
            if m is not None:
                mb, map_ = m
                tr.op("dve", lambda e: e.tensor_mul(out=PT[:, 0:nq], in0=PT[:, 0:nq], in1=map_), R=[PT, mb], W=[PT])
        if ci + LOOK < n:
            issue_qk(ci + LOOK)
        tr.op("pe", lambda e: e.matmul(pO[:, 0:nq], lhsT=V[:, ch, vcol:vcol + 64], rhs=PT[:, 0:nq], start=(ci == 0), stop=(ci == n - 1)),
              R=[V, PT], W=[pO], sig=False)
        tr.op("pe", lambda e: e.matmul(pZ[:, 0:nq], lhsT=ac.ones[:], rhs=PT[:, 0:nq], start=(ci == 0), stop=(ci == n - 1)),
              R=[ac.ones, PT], W=[pZ, pO], sig=True)
        yield
    rinv, yo = ac.rinv[o], ac.yo[o]
    tr.op("dve", lambda e: e.reciprocal(out=rinv[:, 0:nq], in_=pZ[:, 0:nq]), R=[pZ], W=[rinv])
    tr.op("dve", lambda e: e.tensor_mul(out=yo[:, 0:nq], in0=pO[:, 0:nq], in1=rinv[:, 0:nq]), R=[pO, rinv], W=[yo])
    tr.dma("sp", YT[yrow:yrow + 64, ycol:ycol + nq], yo[:, 0:nq], R=[yo], W=[YT])


def stage_gqa(nc, tr, QT, KT, vtm, YT, need_ctx):
    with ExitStack() as es:
        tr.es = es
        ac = AttnCtx(tr)
        V = tr.sb([128, NKC, 128], BF16, "V")
        tr.dma("sp", V[:], vtm[:, 0:128].rearrange("(c p) n -> p c n", p=128), R=[vtm], W=[V])
        Kg = [tr.sb([64, NTOK], BF16, f"K{g}") for g in range(2)]
        for g in range(2):
            tr.dma("sp", Kg[g][:], KT[g * 64:(g + 1) * 64, :], R=[KT], W=[Kg[g]])
        Qh = [tr.sb([64, NOWN + NCTX], BF16, f"Q{i}") for i in range(2)]
        allchunks = list(range(NKC))
        ctxchunks = [32, 33]
        gens = []
        for hq in range(8):
            q = Qh[hq % 2]
            g = hq // 4

            def pre(q=q, hq=hq):
                tr.dma("sp", q[:, 0:NOWN], QT[hq * 64:(hq + 1) * 64, 0:NOWN], R=[QT], W=[q])
                if need_ctx:
                    tr.dma("sp", q[:, NOWN:NOWN + NCTX], QT[hq * 64:(hq + 1) * 64, NLAT:NLAT + NCTX], R=[QT], W=[q])
            for qb in range(4):
                gens.append(attn_block_gen(tr, ac, q, qb * 512, 512, Kg[g], V, g * 64, allchunks, YT, hq * 64, qb * 512, pre=(pre if qb == 0 else None)))
            if need_ctx:
                gens.append(attn_block_gen(tr, ac, q, NOWN, NCTX, Kg[g], V, g * 64, ctxchunks, YT, hq * 64, NOWN))
        interleave(gens, width=1)
        tr.barrier()


def na_slots(u):
    if 2 <= u <= 13:
        return [(s, s - 1) for s in range(1, 6)]
    spec = {0: 0, 1: 1, 14: 2, 15: 3}[u]
    return [(s, 5 + spec * 7 + s) for s in range(7)]


def stage_na(nc, tr, projT, vtm, nb_bias, YT, need_ctx):
    with ExitStack() as es:
        tr.es = es
        ac = AttnCtx(tr)
        V = tr.sb([128, NKC, 256], BF16, "NV")
        tr.dma("sp", V[:], vtm[:, 128:384].rearrange("(c p) n -> p c n", p=128), R=[vtm], W=[V])
        EB = tr.sb([128, 4, 33, 128], BF16, "EB")
        stg = [tr.sb([128, 11, 128], F32, f"stg{i}") for i in range(2)]
        k = 0
        for h in range(4):
            for part in range(3):
                st = stg[k % 2]
                k += 1
                tr.dma("sp", st[:], nb_bias[h, part * 11:(part + 1) * 11, :, :].rearrange("s k q -> k s q"), R=[nb_bias], W=[st])
                tr.op("act", lambda e: e.activation(out=EB[:, h, part * 11:(part + 1) * 11, :], in_=st[:], func=AF.Exp), R=[st], W=[EB])
        Kh = [tr.sb([64, NTOK], BF16, f"NK{i}") for i in range(2)]
        Qh = [tr.sb([64, NOWN + NCTX], BF16, f"NQ{i}") for i in range(2)]
        gens = []
        for h in range(4):
            kk = Kh[h % 2]; q = Qh[h % 2]

            def pre(kk=kk, q=q, h=h):
                tr.dma("pool", kk[:], projT[1792 + h * 64:1792 + (h + 1) * 64, :], R=[projT], W=[kk])
                tr.dma("pool", q[:, 0:NOWN], projT[1536 + h * 64:1536 + (h + 1) * 64, 0:NOWN], R=[projT], W=[q])
                if need_ctx:
                    tr.dma("pool", q[:, NOWN:NOWN + NCTX], projT[1536 + h * 64:1536 + (h + 1) * 64, NLAT:NLAT + NCTX], R=[projT], W=[q])
            for u in range(16):
                slots = na_slots(u)
                chunks = [32, 33] + [(u + s - 3) % 32 for s, _ in slots]

                def mask_fn(ci, slots=slots, h=h):
                    if ci < 2:
                        return None
                    return EB, EB[:, h, slots[ci - 2][1], :]
                gens.append(attn_block_gen(tr, ac, q, u * 128, 128, kk, V, h * 64, chunks, YT, 768 + h * 64, u * 128, mask_fn=mask_fn, pre=(pre if u == 0 else None)))
            if need_ctx:
                gens.append(attn_block_gen(tr, ac, q, NOWN, NCTX, kk, V, h * 64, [32, 33], YT, 768 + h * 64, NOWN))
        interleave(gens, width=1)
        tr.barrier()

import math

PI = math.pi


def _wrap(tr, a, t, n):
    for _ in range(2):
        tr.op("dve", lambda e: e.tensor_scalar(out=t[:, 0:n], in0=a[:, 0:n], scalar1=PI, scalar2=-2 * PI, op0=ALU.is_gt, op1=ALU.mult), R=[a], W=[t])
        tr.op("dve", lambda e: e.tensor_add(out=a[:, 0:n], in0=a[:, 0:n], in1=t[:, 0:n]), R=[a, t], W=[a])
        tr.op("dve", lambda e: e.tensor_scalar(out=t[:, 0:n], in0=a[:, 0:n], scalar1=-PI, scalar2=2 * PI, op0=ALU.is_lt, op1=ALU.mult), R=[a], W=[t])
        tr.op("dve", lambda e: e.tensor_add(out=a[:, 0:n], in0=a[:, 0:n], in1=t[:, 0:n]), R=[a, t], W=[a])


def hyena_filter(nc, tr, P, ZF, ZB, DECF, DECB, L, nlag, GA, GB, sel):
    with ExitStack() as es:
        tr.es = es
        w1 = tr.sb([33, 64], F32, "w1"); w2 = tr.sb([64, 64], F32, "w2"); w3 = tr.sb([64, 512], F32, "w3")
        tr.dma("sp", w1[:], P["f_w1"][:], R=[P["f_w1"]], W=[w1])
        tr.dma("sp", w2[:], P["f_w2"][:], R=[P["f_w2"]], W=[w2])
        tr.dma("sp", w3[:], P["f_w3"][:], R=[P["f_w3"]], W=[w3])
        col = lambda ap: ap.rearrange("(p o) -> p o", o=1)
        fr = tr.sb([64, 2], F32, "fr"); bb = tr.sb([64, 2], F32, "bb"); fb = tr.sb([64, 2], F32, "fb")
        for i in range(2):
            tr.dma("sp", fr[:, i:i + 1], col(P["f_freq"][i, :]), R=[P["f_freq"]], W=[fr])
        tr.dma("sp", bb[:, 0:1], col(P["f_b1"][:]), R=[P["f_b1"]], W=[bb])
        tr.dma("sp", bb[:, 1:2], col(P["f_b2"][:]), R=[P["f_b2"]], W=[bb])
        tr.op("dve", lambda e: e.tensor_mul(out=fb[:], in0=fr[:], in1=bb[:]), R=[fr, bb], W=[fb])
        b3 = tr.sb([128, 4], F32, "b3")
        for i in range(4):
            tr.dma("sp", b3[:, i:i + 1], col(P["f_b3"][i * 128:(i + 1) * 128]), R=[P["f_b3"]], W=[b3])
        Z = [tr.sb([33, L], F32, "ZF"), tr.sb([33, L], F32, "ZB")]
        tr.dma("sp", Z[0][:], ZF[:], R=[ZF], W=[Z[0]])
        tr.dma("sp", Z[1][:], ZB[:], R=[ZB], W=[Z[1]])
        H = [[tr.sb([128, L], F32, f"H{d}{c}") for c in range(2)] for d in range(2)]
        BLK = min(512, L)
        fbuf = [dict(a=[tr.sb([64, BLK], F32, f"a{i}{r}") for i in range(2)], t=[tr.sb([64, BLK], F32, f"t{i}{r}") for i in range(2)],
                     h1=tr.sb([64, BLK], F32, f"h1{r}"), h2=tr.sb([64, BLK], F32, f"h2{r}"),
                     dec=[tr.sb([128, BLK], F32, f"dec{i}{r}") for i in range(2)],
                     p1=tr.ps([64, BLK], F32, f"p1{r}"), p2=tr.ps([64, BLK], F32, f"p2{r}"),
                     p3=[tr.ps([128, BLK], F32, f"p3{i}{r}") for i in range(2)]) for r in range(2)]
        DEC = [DECF, DECB]
        bi = 0
        for d in range(2):
            for b in range(L // BLK):
                sl = slice(b * BLK, (b + 1) * BLK)
                fb_ = fbuf[bi % 2]
                bi += 1
                a = fb_["a"]; h1 = fb_["h1"]; h2 = fb_["h2"]; dec = fb_["dec"]; p1 = fb_["p1"]; p2 = fb_["p2"]; p3 = fb_["p3"]
                tr.op("pe", lambda e: e.matmul(p1[:], lhsT=w1[:], rhs=Z[d][:, sl], start=True, stop=True), R=[w1, Z[d]], W=[p1])
                tr.op("dve", lambda e: e.tensor_scalar(out=a[0][:], in0=p1[:], scalar1=fr[:, 0:1], scalar2=fb[:, 0:1], op0=ALU.mult, op1=ALU.add), R=[p1, fr, fb], W=[a[0]])
                _wrap(tr, a[0], fb_['t'][0], BLK)
                tr.op("act", lambda e: e.activation(out=h1[:], in_=a[0][:], func=AF.Sin), R=[a[0]], W=[h1])
                tr.op("pe", lambda e: e.matmul(p2[:], lhsT=w2[:], rhs=h1[:], start=True, stop=True), R=[w2, h1], W=[p2])
                tr.op("dve", lambda e: e.tensor_scalar(out=a[1][:], in0=p2[:], scalar1=fr[:, 1:2], scalar2=fb[:, 1:2], op0=ALU.mult, op1=ALU.add), R=[p2, fr, fb], W=[a[1]])
                _wrap(tr, a[1], fb_['t'][1], BLK)
                tr.op("act", lambda e: e.activation(out=h2[:], in_=a[1][:], func=AF.Sin), R=[a[1]], W=[h2])
                for c in range(2):
                    tr.dma("sp", dec[c][:], DEC[d][c * 128:(c + 1) * 128, sl], R=[DEC[d]], W=[dec[c]])
                    tr.op("pe", lambda e: e.matmul(p3[c][:], lhsT=w3[:, d * 256 + c * 128:d * 256 + (c + 1) * 128], rhs=h2[:], start=True, stop=True), R=[w3, h2], W=[p3[c]])
                    tr.op("dve", lambda e: e.scalar_tensor_tensor(out=H[d][c][:, sl], in0=p3[c][:], scalar=b3[:, d * 2 + c:d * 2 + c + 1], in1=dec[c][:], op0=ALU.add, op1=ALU.mult),
                          R=[p3[c], b3, dec[c]], W=[H[d][c]])
        junk = tr.sb([128, L], BF16, "junk")
        ss = tr.sb([128, 4], F32, "ss")
        tot = tr.sb([128, 2], F32, "tot"); sd = tr.sb([128, 2], F32, "sd"); nrm = tr.sb([128, 2], F32, "nrm")
        n0 = tr.sb([128, 2], F32, "n0"); n1 = tr.sb([128, 2], F32, "n1")
        selt = tr.sb([128, 2], F32, "selt")
        tr.dma("sp", selt[:], sel[:], R=[sel], W=[selt])
        for d in range(2):
            for c in range(2):
                tr.op("act", lambda e: e.activation(out=junk[:], in_=H[d][c][:], func=AF.Square, accum_out=ss[:, d * 2 + c:d * 2 + c + 1]), R=[H[d][c]], W=[junk, ss])
        tr.op("dve", lambda e: e.tensor_add(out=tot[:], in0=ss[:, 0:2], in1=ss[:, 2:4]), R=[ss], W=[tot])
        tr.op("act", lambda e: e.activation(out=sd[:], in_=tot[:], func=AF.Sqrt, bias=EPS), R=[tot], W=[sd])
        tr.op("dve", lambda e: e.reciprocal(out=nrm[:], in_=sd[:]), R=[sd], W=[nrm])
        tr.op("dve", lambda e: e.tensor_scalar_mul(out=n0[:], in0=nrm[:], scalar1=selt[:, 0:1]), R=[nrm, selt], W=[n0])
        tr.op("dve", lambda e: e.tensor_scalar_mul(out=n1[:], in0=nrm[:], scalar1=selt[:, 1:2]), R=[nrm, selt], W=[n1])
        WA = GA.t.shape[1]
        for c in range(2):
            ga = tr.sb([128, WA], BF16, f"ga{c}")
            tr.op("pool", lambda e: e.memset(ga[:], 0.0), W=[ga])
            tr.op("dve", lambda e: e.tensor_scalar_mul(out=ga[:, 0:nlag + 1], in0=H[0][c][:, L - 1 - nlag:L], scalar1=nrm[:, c:c + 1]), R=[H[0][c], nrm], W=[ga])
            tr.op("dve", lambda e: e.tensor_scalar_mul(out=ga[:, nlag + 1:2 * nlag + 1], in0=H[1][c][:, 1:nlag + 1], scalar1=nrm[:, c:c + 1]), R=[H[1][c], nrm], W=[ga])
            tr.dma("sp", GA[c * 128:(c + 1) * 128, :], ga[:], R=[ga], W=[GA])
            if GB is not None:
                tmp = tr.sb([128, L], F32, f"gtmp{c}")
                gb = tr.sb([128, L], BF16, f"gb{c}")
                tr.op("pool", lambda e: e.memset(gb[:], 0.0), W=[gb])
                tr.op("dve", lambda e: e.tensor_scalar_mul(out=tmp[:, 0:L - 1], in0=H[1][c][:, 1:L], scalar1=n0[:, c:c + 1]), R=[H[1][c], n0], W=[tmp])
                tr.op("dve", lambda e: e.scalar_tensor_tensor(out=gb[:, 0:L - 1], in0=H[0][c][:, 0:L - 1], scalar=n1[:, c:c + 1], in1=tmp[:, 0:L - 1], op0=ALU.mult, op1=ALU.add),
                      R=[H[0][c], n1, tmp], W=[gb])
                tr.dma("sp", GB[c * 128:(c + 1) * 128, :], gb[:], R=[gb], W=[GB])
        tr.barrier()


def toeplitz_conv(tr, NBo, srcs, Yc, pY, NCOL=None):
    W = (2 * NBo - 1) * 128
    NCOL = NCOL or NBo
    nper = 512 // NCOL
    strips = [[tr.sb([128, W], BF16, f"strip{si}_{i}") for i in range(3)] for si in range(len(srcs))]
    nmm = len(srcs) * (2 * NBo - 1)
    for c in range(256):
        bank = pY[(c // nper) % 2]
        k = 0
        for si, (G, Z) in enumerate(srcs):
            st = strips[si][c % 3]
            rowlen = G.t.shape[1]
            src = bass.AP(G.t.tensor, c * rowlen, [[1, 128], [1, W]])
            tr.dma("sp" if si == 0 else "act", st[:], src, R=[G], W=[st])
            for d in range(-(NBo - 1), NBo):
                tr.op("pe", lambda e: e.matmul(bank[:, (c % nper) * NCOL:(c % nper + 1) * NCOL], lhsT=st[:, (NBo - 1 - d) * 128:(NBo - d) * 128],
                                               rhs=Z[:, c, NBo - 1 - d:NBo - 1 - d + NCOL], start=(k == 0), stop=(k == nmm - 1)),
                      R=[st, Z], W=[bank], sig=(k == nmm - 1))
                k += 1
        if c % nper == nper - 1:
            c0 = c - nper + 1
            tr.op("dve", lambda e: e.tensor_copy(out=Yc[:, c0:c0 + nper, :], in_=bank[:, 0:512].rearrange("p (c i) -> p c i", i=NCOL)), R=[bank], W=[Yc])


def stage_hyena(nc, tr, projT, P, tabs, sel, GA, GB, GAc, YT, ident_bf, rev_f, need_ctx, stop_after=None):
    hyena_filter(nc, tr, P, tabs["ZF"], tabs["ZB"], tabs["DECF"], tabs["DECB"], 4096, 2047, GA, GB, sel)
    if need_ctx:
        hyena_filter(nc, tr, P, tabs["ZFc"], tabs["ZBc"], tabs["DECFc"], tabs["DECBc"], 256, 255, GAc, None, sel)
    NX = NTOK if need_ctx else NLAT
    NO = NQ if need_ctx else NOWN
    if stop_after == "filter":
        return
    with ExitStack() as es:
        tr.es = es
        col = lambda ap: ap.rearrange("(p o) -> p o", o=1)
        cw = tr.sb([128, 6, 3], F32, "cw"); cb = tr.sb([128, 6], F32, "cb")
        for cc in range(6):
            for j in range(3):
                tr.dma("sp", cw[:, cc, j:j + 1], col(P["conv_w"][j, cc * 128:(cc + 1) * 128]), R=[P["conv_w"]], W=[cw])
            tr.dma("sp", cb[:, cc:cc + 1], col(P["conv_b"][cc * 128:(cc + 1) * 128]), R=[P["conv_b"]], W=[cb])
        selt = tr.sb([128, 2], F32, "selt")
        tr.dma("sp", selt[:], sel[:], R=[sel], W=[selt])
        cwh = tr.sb([128, 6, 3], F32, "cwh"); ncwh = tr.sb([128, 6, 3], F32, "ncwh"); ncw = tr.sb([128, 6, 3], F32, "ncw")
        tr.op("dve", lambda e: e.tensor_scalar_mul(out=cwh[:], in0=cw[:], scalar1=selt[:, 1:2]), R=[cw, selt], W=[cwh])
        tr.op("dve", lambda e: e.tensor_scalar_mul(out=ncwh[:], in0=cwh[:], scalar1=-1.0), R=[cwh], W=[ncwh])
        tr.op("dve", lambda e: e.tensor_scalar_mul(out=ncw[:], in0=cw[:], scalar1=-1.0), R=[cw], W=[ncw])
        dsk = tr.sb([128, 2], F32, "dsk")
        for c in range(2):
            tr.dma("sp", dsk[:, c:c + 1], col(P["d_skip"][c * 128:(c + 1) * 128]), R=[P["d_skip"]], W=[dsk])
        zb = tr.sb([128, 2, NX], BF16, "zb")
        zf = tr.sb([128, 2, NO], F32, "zf")
        x0 = tr.sb([128, 2, NO], F32, "x0")
        Zown = tr.sb([128, 256, 46], BF16, "Zown")
        Zoth = tr.sb([128, 256, 46], BF16, "Zoth")
        tr.op("pool", lambda e: e.memset(Zown[:], 0.0), W=[Zown])
        tr.op("pool", lambda e: e.memset(Zoth[:], 0.0), W=[Zoth])
        if need_ctx:
            Zc = tr.sb([128, 256, 18], BF16, "Zc")
            tr.op("pool", lambda e: e.memset(Zc[:], 0.0), W=[Zc])
        with ExitStack() as es2:
            tr.es = es2
            U = tr.sb([128, NX], F32, "U")
            Cs = [tr.sb([128, NX], F32, f"C{i}") for i in range(2)]

            def fix(C, dcol, scol, wt, cc, j):
                tr.op("dve", lambda e: e.scalar_tensor_tensor(out=C[:, dcol:dcol + 1], in0=U[:, scol:scol + 1], scalar=wt[:, cc, j:j + 1], in1=C[:, dcol:dcol + 1], op0=ALU.mult, op1=ALU.add),
                      R=[U, wt, C], W=[C])

            def sconv(cc, C):
                tr.dma("sp", U[:], projT[768 + cc * 128:768 + (cc + 1) * 128, 0:NX], R=[projT], W=[U])
                tr.op("act", lambda e: e.activation(out=C[:], in_=U[:], func=AF.Identity, scale=cw[:, cc, 1:2], bias=cb[:, cc:cc + 1]), R=[U, cw, cb], W=[C])
                tr.op("dve", lambda e: e.scalar_tensor_tensor(out=C[:, 1:NX], in0=U[:, 0:NX - 1], scalar=cw[:, cc, 0:1], in1=C[:, 1:NX], op0=ALU.mult, op1=ALU.add), R=[U, cw, C], W=[C])
                tr.op("dve", lambda e: e.scalar_tensor_tensor(out=C[:, 0:NX - 1], in0=U[:, 1:NX], scalar=cw[:, cc, 2:3], in1=C[:, 0:NX - 1], op0=ALU.mult, op1=ALU.add), R=[U, cw, C], W=[C])
                fix(C, 2048, 2047, ncwh, cc, 0); fix(C, 2047, 2048, ncwh, cc, 2)
                fix(C, 0, 4095, cwh, cc, 0); fix(C, 4095, 0, cwh, cc, 2)
                if need_ctx:
                    fix(C, 4096, 4095, ncw, cc, 0); fix(C, 4095, 4096, ncw, cc, 2)

            for c in range(2):
                sconv(c, Cs[0])
                tr.op("pool", lambda e: e.tensor_copy(out=x0[:, c, 0:NOWN], in_=Cs[0][:, 0:NOWN]), R=[Cs[0]], W=[x0])
                if need_ctx:
                    tr.op("pool", lambda e: e.tensor_copy(out=x0[:, c, NOWN:NO], in_=Cs[0][:, NLAT:NX]), R=[Cs[0]], W=[x0])
                sconv(2 + c, Cs[0])
                sconv(4 + c, Cs[1])
                tr.op("dve", lambda e: e.tensor_mul(out=zb[:, c, :], in0=Cs[0][:], in1=Cs[1][:]), R=Cs, W=[zb])
                tr.op("pool", lambda e: e.tensor_mul(out=zf[:, c, 0:NOWN], in0=Cs[0][:, 0:NOWN], in1=Cs[1][:, 0:NOWN]), R=Cs, W=[zf])
                if need_ctx:
                    tr.op("pool", lambda e: e.tensor_mul(out=zf[:, c, NOWN:NO], in0=Cs[0][:, NLAT:NX], in1=Cs[1][:, NLAT:NX]), R=Cs, W=[zf])
            tr.barrier()
        tr.es = es
        if stop_after == "sconv":
            dbg = tr.sb([128, 512], BF16, "dbg")
            for c in range(2):
                tr.op("dve", lambda e: e.tensor_copy(out=dbg[:], in_=zf[:, c, 0:512]), R=[zf], W=[dbg])
                tr.dma("sp", YT[512 + c * 128:512 + (c + 1) * 128, 0:512], dbg[:], R=[dbg], W=[YT])
            tr.barrier()
            return
        pT = [tr.ps([128, 8, 128], BF16, f"hpT{i}") for i in range(2)]
        k = 0
        groups = [(Zown, 15, 0, 8), (Zown, 23, 8, 8), (Zoth, 15, 16, 8), (Zoth, 23, 24, 8)]
        if need_ctx:
            groups.append((Zc, 1, 32, 2))
        for (Zt, pad0, blk0, nb) in groups:
            for c in range(2):
                p = pT[k % 2]
                k += 1
                for j in range(nb):
                    tr.op("pe", lambda e: e.transpose(out=p[:, j, :], in_=zb[:, c, (blk0 + j) * 128:(blk0 + j + 1) * 128], identity=ident_bf[:]),
                          R=[zb, ident_bf], W=[p], sig=(j == nb - 1))
                tr.op("dve", lambda e: e.tensor_copy(out=Zt[:, c * 128:(c + 1) * 128, pad0:pad0 + nb].rearrange("p c j -> p j c"), in_=p[:, 0:nb, :]), R=[p], W=[Zt])
        if stop_after == "ztrans":
            tr.barrier()
            return
        pY = [tr.ps([128, 512], F32, f"pY{i}") for i in range(2)]
        pB = [tr.ps([128, 4, 128], F32, f"pBk{i}") for i in range(2)]
        tmp = [tr.sb([128, 512], F32, f"ytmp{i}") for i in range(2)]
        yo = [tr.sb([128, 512], BF16, f"yo{i}") for i in range(2)]

        def back(Yc, NBo, col0):
            kk = 0
            for c in range(2):
                for i0 in range(0, NBo, 4):
                    nb = min(4, NBo - i0)
                    p = pB[kk % 2]; t_ = tmp[kk % 2]; y_ = yo[kk % 2]
                    kk += 1
                    n = nb * 128
                    for j in range(nb):
                        tr.op("pe", lambda e: e.matmul(p[:, j, :], lhsT=Yc[:, c * 128:(c + 1) * 128, i0 + j], rhs=rev_f[:], start=True, stop=True),
                              R=[Yc, rev_f], W=[p], sig=(j == nb - 1))
                    cs = slice(col0 + i0 * 128, col0 + i0 * 128 + n)
                    tr.op("dve", lambda e: e.scalar_tensor_tensor(out=t_[:, 0:n], in0=zf[:, c, cs], scalar=dsk[:, c:c + 1], in1=p[:, 0:nb, :].rearrange("p j i -> p (j i)"), op0=ALU.mult, op1=ALU.add),
                          R=[zf, dsk, p], W=[t_])
                    tr.op("dve", lambda e: e.tensor_mul(out=y_[:, 0:n], in0=t_[:, 0:n], in1=x0[:, c, cs]), R=[t_, x0], W=[y_])
                    tr.dma("sp", YT[512 + c * 128:512 + (c + 1) * 128, cs], y_[:, 0:n], R=[y_], W=[YT])

        with ExitStack() as es3:
            tr.es = es3
            Yc = tr.sb([128, 256, 16], F32, "Yc")
            toeplitz_conv(tr, 16, [(GA, Zown), (GB, Zoth)], Yc, pY)
            if stop_after != "toep":
                back(Yc, 16, 0)
            tr.barrier()
        if need_ctx:
            with ExitStack() as es4:
                tr.es = es4
                Ycc = tr.sb([128, 256, 16], F32, "Ycc")
                toeplitz_conv(tr, 2, [(GAc, Zc)], Ycc, pY, NCOL=16)
                if stop_after != "toep":
                    back(Ycc, 2, NOWN)
                tr.barrier()
        tr.es = es
        tr.barrier()


ALPHA = (2.0 * 2) ** 0.25


def load_rep(tr, src_row_ap, srcbuf, n=1024, name="rep"):
    t = tr.sb([128, n], F32, name)
    tr.dma("sp", t[:], src_row_ap.partition_broadcast(128), R=[srcbuf], W=[t])
    return t


def ln_affine_store(tr, u, stats, mv, rstd, tmp, nmr, lng, lnb, dst, row0):
    ln_stats(tr, u, stats, mv, rstd, tmp)
    tr.op("dve", lambda e: e.scalar_tensor_tensor(out=nmr[:], in0=mv[:, 0:1], scalar=-1.0, in1=rstd[:], op0=ALU.mult, op1=ALU.mult), R=[mv, rstd], W=[nmr])
    tr.op("act", lambda e: e.activation(out=u[:], in_=u[:], func=AF.Identity, scale=rstd[:], bias=nmr[:]), R=[u, rstd, nmr], W=[u])
    tr.op("pool", lambda e: e.tensor_mul(out=u[:], in0=u[:], in1=lng[:]), R=[u, lng], W=[u])
    tr.op("dve", lambda e: e.tensor_add(out=u[:], in0=u[:], in1=lnb[:]), R=[u, lnb], W=[u])
    tr.dma("sp", dst[row0:row0 + 128, :], u[:], R=[u], W=[dst])


def stage_outproj(nc, tr, YT, w_out, xsrc, modrow, ln_g, ln_b, X1, need_ctx):
    NO = NQ if need_ctx else NOWN
    with ExitStack() as es:
        tr.es = es
        w = tr.sb([128, 8, D], BF16, "w_out")
        for k in range(8):
            tr.dma("pool", w[:, k, :], w_out[k * 128:(k + 1) * 128, :], R=[w_out], W=[w])
        g1 = [load_rep(tr, modrow[r, 2048:3072], modrow, name=f"g1_{r}") for r in range(2)]
        lng = load_rep(tr, ln_g[:], ln_g, name="lng"); lnb = load_rep(tr, ln_b[:], ln_b, name="lnb")
        yT = [tr.sb([128, 8, 512], BF16, f"yT{i}") for i in range(2)]
        xt = [tr.sb([128, D], F32, f"xo{i}") for i in range(2)]
        u = [tr.sb([128, D], F32, f"u{i}") for i in range(2)]
        po = [[tr.ps([128, 512], F32, f"po{i}{j}") for j in range(2)] for i in range(2)]
        lnb_ = [[tr.sb([128, 12], F32, f"stats{i}"), tr.sb([128, 2], F32, f"mv{i}"), tr.sb([128, 1], F32, f"rstd{i}"),
                 tr.sb([128, 1], F32, f"tmp{i}"), tr.sb([128, 1], F32, f"nmr{i}")] for i in range(2)]
        it = 0
        for b0 in range(0, NO, 512):
            nb = min(512, NO - b0)
            y = yT[(b0 // 512) % 2]
            for k in range(8):
                tr.dma("sp", y[:, k, 0:nb], YT[k * 128:(k + 1) * 128, b0:b0 + nb], R=[YT], W=[y])
            for ti in range(nb // 128):
                tok = b0 + ti * 128
                r = 1 if tok >= NOWN else 0
                srow = tok if r == 0 else NLAT + (tok - NOWN)
                x = xt[it % 2]; uu = u[it % 2]; pp = po[it % 2]
                stats, mv, rstd, tmp, nmr = lnb_[it % 2]
                it += 1
                tr.dma("sp", x[:], xsrc[srow:srow + 128, :], R=[xsrc], W=[x])
                for half in range(2):
                    for k in range(8):
                        tr.op("pe", lambda e: e.matmul(pp[half][:], lhsT=y[:, k, ti * 128:(ti + 1) * 128], rhs=w[:, k, half * 512:(half + 1) * 512],
                                                       start=(k == 0), stop=(k == 7)), R=[y, w], W=[pp[half]], sig=(k == 7))
                    tr.op("dve", lambda e: e.tensor_mul(out=uu[:, half * 512:(half + 1) * 512], in0=pp[half][:], in1=g1[r][:, half * 512:(half + 1) * 512]),
                          R=[pp[half], g1[r]], W=[uu])
                tr.op("dve", lambda e: e.scalar_tensor_tensor(out=uu[:], in0=x[:], scalar=ALPHA, in1=uu[:], op0=ALU.mult, op1=ALU.add), R=[x, uu], W=[uu])
                ln_affine_store(tr, uu, stats, mv, rstd, tmp, nmr, lng, lnb, X1, tok)
        tr.barrier()


def stage_moe(nc, tr, X1, modrow, router_w, router_b, exp_w1, exp_b1, exp_w2, exp_b2, ln_g, ln_b, XOUT, ident_f, need_ctx, NE=32):
    NO = NQ if need_ctx else NOWN
    NT = NO // 128
    with ExitStack() as es:
        tr.es = es
        h2T = tr.sb([128, 8, NO], BF16, "h2T")
        acc = [tr.sb([128, D], F32, f"acc{t}") for t in range(NT)]
        gates = tr.sb([128, NT, NE], F32, "gates")
        b1T = tr.sb([128, NE * 16], F32, "b1T")
        b1T1 = tr.sb([128, NE * 16], F32, "b1T1")
        with ExitStack() as es1:
            tr.es = es1
            mods = []
            for r in range(2):
                mT = load_modT(nc, tr, modrow, r, None)
                sp1 = tr.sb([128, 8], F32, "sp1")
                tr.op("dve", lambda e: e.tensor_scalar_add(out=sp1[:], in0=mT[:, 32:40], scalar1=1.0), R=[mT], W=[sp1])
                mods.append((mT, sp1))
            rw = tr.sb([128, 8, NE], F32, "rw")
            tr.dma("sp", rw[:], router_w[:, :].rearrange("(k p) n -> p k n", p=128), R=[router_w], W=[rw])
            rb = tr.sb([128, NE], F32, "rb")
            tr.dma("sp", rb[:], router_b[:].partition_broadcast(128), R=[router_b], W=[rb])
            b2 = tr.sb([NE, D], F32, "b2")
            tr.dma("sp", b2[:], exp_b2[:, :], R=[exp_b2], W=[b2])
            GT = tr.sb([NE, NO], F32, "GT")
            pT = [tr.ps([128, 4, 128], F32, f"mpT{i}") for i in range(2)]
            pR = tr.ps([128, 512], F32, "pR")
            pG = tr.ps([128, 512], F32, "pGt")
            pA = [tr.ps([128, 512], F32, f"pA{i}") for i in range(2)]
            nblk = NE * 16 // 128
            b1raw = tr.sb([128, nblk, 128], F32, "b1raw")
            tr.dma("sp", b1raw[:], exp_b1[:, :].rearrange("e (c p) -> (e c) p", p=128).rearrange("(b r) p -> r b p", r=128), R=[exp_b1], W=[b1raw])
            for bk in range(nblk):
                tr.op("pe", lambda e: e.transpose(out=pR[:, 0:128], in_=b1raw[:, bk, :], identity=ident_f[:]), R=[b1raw, ident_f], W=[pR])
                tr.op("dve", lambda e: e.tensor_copy(out=b1T[:, bk * 128:(bk + 1) * 128], in_=pR[:, 0:128]), R=[pR], W=[b1T])
            tr.op("dve", lambda e: e.tensor_scalar_add(out=b1T1[:], in0=b1T[:], scalar1=1.0), R=[b1T], W=[b1T1])
            xt = [tr.sb([128, D], F32, f"mx{i}") for i in range(2)]
            xn = [tr.sb([128, D], F32, f"mxn{i}") for i in range(2)]
            hf = [tr.sb([128, 8, 128], F32, f"hf{i}") for i in range(2)]
            lnb_ = [[tr.sb([128, 12], F32, f"stats{i}"), tr.sb([128, 2], F32, f"mv{i}"), tr.sb([128, 1], F32, f"rstd{i}"),
                     tr.sb([128, 1], F32, f"tmp{i}"), tr.sb([128, 1], F32, f"nmr{i}")] for i in range(2)]
            rtb_ = [[tr.sb([128, NE], F32, f"lg{i}"), tr.sb([128, 8], F32, f"m8{i}"), tr.sb([128, 1], F32, f"nmx{i}"), tr.sb([128, NE], F32, f"msk{i}"),
                     tr.sb([128, NE], F32, f"ex{i}"), tr.sb([128, 1], F32, f"ssum{i}"), tr.sb([128, 1], F32, f"rs{i}")] for i in range(2)]
            for t in range(NT):
                tok = t * 128
                stats, mv, rstd, tmp, nmr = lnb_[t % 2]
                lg, m8, nmx, msk, ex, ssum, rs = rtb_[t % 2]
                r = 1 if tok >= NOWN else 0
                mT, sp1 = mods[r]
                x = xt[t % 2]; xx = xn[t % 2]; h = hf[t % 2]
                tr.dma("sp", x[:], X1[tok:tok + 128, :], R=[X1], W=[x])
                ln_stats(tr, x, stats, mv, rstd, tmp)
                tr.op("dve", lambda e: e.scalar_tensor_tensor(out=nmr[:], in0=mv[:, 0:1], scalar=-1.0, in1=rstd[:], op0=ALU.mult, op1=ALU.mult), R=[mv, rstd], W=[nmr])
                tr.op("act", lambda e: e.activation(out=xx[:], in_=x[:], func=AF.Identity, scale=rstd[:], bias=nmr[:]), R=[x, rstd, nmr], W=[xx])
                for c in range(8):
                    p = pT[(c // 4) % 2]
                    tr.op("pe", lambda e: e.transpose(out=p[:, c % 4, :], in_=xx[:, c * 128:(c + 1) * 128], identity=ident_f[:]), R=[xx, ident_f], W=[p])
                    tr.op("dve" if c % 2 else "act",
                          (lambda e: e.tensor_scalar(out=h[:, c, :], in0=p[:, c % 4, :], scalar1=sp1[:, c:c + 1], scalar2=mT[:, 24 + c:25 + c], op0=ALU.mult, op1=ALU.add)) if c % 2 else
                          (lambda e: e.activation(out=h[:, c, :], in_=p[:, c % 4, :], func=AF.Identity, scale=sp1[:, c:c + 1], bias=mT[:, 24 + c:25 + c])),
                          R=[p, sp1, mT], W=[h])
                tr.op("pool", lambda e: e.tensor_copy(out=h2T[:, :, tok:tok + 128], in_=h[:]), R=[h], W=[h2T])
                for c in range(8):
                    tr.op("pe", lambda e: e.matmul(pR[:, 0:NE], lhsT=h[:, c, :], rhs=rw[:, c, :], start=(c == 0), stop=(c == 7)), R=[h, rw], W=[pR], sig=(c == 7))
                tr.op("dve", lambda e: e.tensor_add(out=lg[:], in0=pR[:, 0:NE], in1=rb[:]), R=[pR, rb], W=[lg])
                tr.op("dve", lambda e: e.max(out=m8[:], in_=lg[:]), R=[lg], W=[m8])
                tr.op("dve", lambda e: e.tensor_scalar(out=msk[:], in0=lg[:], scalar1=m8[:, 3:4], scalar2=None, op0=ALU.is_ge), R=[lg, m8], W=[msk])
                tr.op("dve", lambda e: e.tensor_scalar_mul(out=nmx[:], in0=m8[:, 0:1], scalar1=-1.0), R=[m8], W=[nmx])
                tr.op("act", lambda e: e.activation(out=ex[:], in_=lg[:], func=AF.Exp, bias=nmx[:]), R=[lg, nmx], W=[ex])
                tr.op("dve", lambda e: e.tensor_mul(out=ex[:], in0=ex[:], in1=msk[:]), R=[ex, msk], W=[ex])
                tr.op("dve", lambda e: e.reduce_sum(out=ssum[:], in_=ex[:], axis=AX.X), R=[ex], W=[ssum])
                tr.op("dve", lambda e: e.reciprocal(out=rs[:], in_=ssum[:]), R=[ssum], W=[rs])
                tr.op("dve", lambda e: e.tensor_scalar_mul(out=gates[:, t, :], in0=ex[:], scalar1=rs[:, 0:1]), R=[ex, rs], W=[gates])
                tr.op("pe", lambda e: e.transpose(out=pG[0:NE, 0:128], in_=gates[:, t, :], identity=ident_f[:]), R=[gates, ident_f], W=[pG])
                tr.op("dve", lambda e: e.tensor_copy(out=GT[:, tok:tok + 128], in_=pG[0:NE, 0:128]), R=[pG], W=[GT])
                for half in range(2):
                    pa = pA[half]
                    tr.op("pe", lambda e: e.matmul(pa[:], lhsT=GT[:, tok:tok + 128], rhs=b2[:, half * 512:(half + 1) * 512], start=True, stop=True), R=[GT, b2], W=[pa])
                    tr.op("act", lambda e: e.activation(out=acc[t][:, half * 512:(half + 1) * 512], in_=pa[:], func=AF.Copy), R=[pa], W=[acc[t]])
            tr.barrier()
        tr.es = es
        with ExitStack() as es2:
            tr.es = es2
            w1h = [tr.sb([128, 8, 1024], BF16, f"w1h{i}") for i in range(2)]
            w2h = [tr.sb([128, 4, 1024], BF16, f"w2h{i}") for i in range(2)]
            actT = [tr.sb([128, 4, 512], BF16, f"actT{i}") for i in range(2)]
            gsb = [tr.sb([128, 512], BF16, f"g{i}") for i in range(2)]
            sig = [tr.sb([128, 512], BF16, f"sg{i}") for i in range(2)]
            gs = [tr.sb([128, 512], BF16, f"gs{i}") for i in range(2)]
            tl = [tr.sb([128, 512], F32, f"tl{i}") for i in range(2)]
            pGm = [tr.ps([128, 512], F32, f"pGm{i}") for i in range(2)]
            pLm = [tr.ps([128, 512], F32, f"pLm{i}") for i in range(2)]
            pO = [[tr.ps([128, 512], F32, f"pOm{i}{j}") for j in range(2)] for i in range(2)]
            ui = 0
            ei = 0
            oi = 0

            def load_unit(u):
                e_, s = u // 2, u % 2
                w1 = w1h[u % 2]; w2 = w2h[u % 2]
                tr.dma("pool", w1[:, :, 0:512], exp_w1[e_, :, s * 512:(s + 1) * 512].rearrange("(k p) n -> p k n", p=128), R=[exp_w1], W=[w1])
                tr.dma("pool", w1[:, :, 512:1024], exp_w1[e_, :, 1024 + s * 512:1024 + (s + 1) * 512].rearrange("(k p) n -> p k n", p=128), R=[exp_w1], W=[w1])
                tr.dma("pool", w2[:], exp_w2[e_, s * 512:(s + 1) * 512, :].rearrange("(k p) n -> p k n", p=128), R=[exp_w2], W=[w2])

            blocks = [(b0, min(512, NO - b0)) for b0 in range(0, NO, 512)]
            items = [(u, bi) for u in range(2 * NE) for bi in range(len(blocks))]
            state = {"ei": 0, "oi": 0}

            def w1_phase(i):
                u, bi = items[i]
                e_, s = u // 2, u % 2
                b0, nb = blocks[bi]
                w1 = w1h[u % 2]
                aT = actT[i % 2]
                for m in range(4):
                    j = state["ei"] % 2
                    state["ei"] += 1
                    cg = e_ * 16 + s * 4 + m
                    cl = e_ * 16 + 8 + s * 4 + m
                    for k in range(8):
                        tr.op("pe", lambda e: e.matmul(pGm[j][:, 0:nb], lhsT=w1[:, k, m * 128:(m + 1) * 128], rhs=h2T[:, k, b0:b0 + nb], start=(k == 0), stop=(k == 7)),
                              R=[w1, h2T], W=[pGm[j]], sig=(k == 7))
                    for k in range(8):
                        tr.op("pe", lambda e: e.matmul(pLm[j][:, 0:nb], lhsT=w1[:, k, 512 + m * 128:512 + (m + 1) * 128], rhs=h2T[:, k, b0:b0 + nb], start=(k == 0), stop=(k == 7)),
                              R=[w1, h2T], W=[pLm[j]], sig=(k == 7))
                    tr.op("dve", lambda e: e.tensor_scalar(out=gsb[j][:, 0:nb], in0=pGm[j][:, 0:nb], scalar1=b1T[:, cg:cg + 1], scalar2=7.0, op0=ALU.add, op1=ALU.min),
                          R=[pGm[j], b1T], W=[gsb[j]])
                    tr.op("act", lambda e: e.activation(out=sig[j][:, 0:nb], in_=gsb[j][:, 0:nb], func=AF.Sigmoid, scale=1.702), R=[gsb[j]], W=[sig[j]])
                    tr.op("pool", lambda e: e.tensor_mul(out=gs[j][:, 0:nb], in0=gsb[j][:, 0:nb], in1=sig[j][:, 0:nb]), R=[gsb[j], sig[j]], W=[gs[j]])
                    tr.op("dve", lambda e: e.tensor_scalar(out=tl[j][:, 0:nb], in0=pLm[j][:, 0:nb], scalar1=b1T1[:, cl:cl + 1], scalar2=-6.0, op0=ALU.add, op1=ALU.max),
                          R=[pLm[j], b1T1], W=[tl[j]])
                    tr.op("dve", lambda e: e.scalar_tensor_tensor(out=aT[:, m, 0:nb], in0=tl[j][:, 0:nb], scalar=8.0, in1=gs[j][:, 0:nb], op0=ALU.min, op1=ALU.mult),
                          R=[tl[j], gs[j]], W=[aT])

            def w2_phase(i):
                u, bi = items[i]
                e_ = u // 2
                b0, nb = blocks[bi]
                w2 = w2h[u % 2]
                aT = actT[i % 2]
                for ti in range(nb // 128):
                    t = (b0 // 128) + ti
                    pp = pO[state["oi"] % 2]
                    state["oi"] += 1
                    for half in range(2):
                        for m in range(4):
                            tr.op("pe", lambda e: e.matmul(pp[half][:], lhsT=aT[:, m, ti * 128:(ti + 1) * 128], rhs=w2[:, m, half * 512:(half + 1) * 512], start=(m == 0), stop=(m == 3)),
                                  R=[aT, w2], W=[pp[half]], sig=(m == 3))
                        tr.op("dve", lambda e: e.scalar_tensor_tensor(out=acc[t][:, half * 512:(half + 1) * 512], in0=pp[half][:], scalar=gates[:, t, e_:e_ + 1],
                                                                      in1=acc[t][:, half * 512:(half + 1) * 512], op0=ALU.mult, op1=ALU.add),
                              R=[pp[half], gates, acc[t]], W=[acc[t]])

            load_unit(0)
            if 2 * NE > 1:
                load_unit(1)
            w1_phase(0)
            for i in range(len(items)):
                u, bi = items[i]
                if i + 1 < len(items):
                    w1_phase(i + 1)
                w2_phase(i)
                if bi == len(blocks) - 1 and u + 2 < 2 * NE:
                    load_unit(u + 2)
            tr.barrier()
        tr.es = es
        with ExitStack() as es3:
            tr.es = es3
            g2 = [load_rep(tr, modrow[r, 5120:6144], modrow, name=f"g2_{r}") for r in range(2)]
            lng = load_rep(tr, ln_g[:], ln_g, name="lng2"); lnb = load_rep(tr, ln_b[:], ln_b, name="lnb2")
            xt = [tr.sb([128, D], F32, f"fx{i}") for i in range(2)]
            u = [tr.sb([128, D], F32, f"fu{i}") for i in range(2)]
            lnb_ = [[tr.sb([128, 12], F32, f"stats{i}"), tr.sb([128, 2], F32, f"mv{i}"), tr.sb([128, 1], F32, f"rstd{i}"),
                     tr.sb([128, 1], F32, f"tmp{i}"), tr.sb([128, 1], F32, f"nmr{i}")] for i in range(2)]
            for t in range(NT):
                tok = t * 128
                r = 1 if tok >= NOWN else 0
                x = xt[t % 2]; uu = u[t % 2]
                stats, mv, rstd, tmp, nmr = lnb_[t % 2]
                tr.dma("sp", x[:], X1[tok:tok + 128, :], R=[X1], W=[x])
                tr.op("pool", lambda e: e.tensor_mul(out=uu[:], in0=acc[t][:], in1=g2[r][:]), R=[acc[t], g2[r]], W=[uu])
                tr.op("dve", lambda e: e.scalar_tensor_tensor(out=uu[:], in0=x[:], scalar=ALPHA, in1=uu[:], op0=ALU.mult, op1=ALU.add), R=[x, uu], W=[uu])
                ln_affine_store(tr, uu, stats, mv, rstd, tmp, nmr, lng, lnb, XOUT, tok)
            tr.barrier()
        tr.es = es
        tr.barrier()


GRID_W = 64


def local_token_ids(h):
    own = np.arange(2048) + 2048 * h
    oth = np.arange(2048) + 2048 * (1 - h)
    return np.concatenate([own, oth])


def rope_tables(h):
    t = local_token_ids(h)
    row = (t // GRID_W).astype(np.float32)[:, None]
    col = (t % GRID_W).astype(np.float32)[:, None]
    inv_freq = (np.float32(10000.0) ** (-np.arange(0, 32, 2, dtype=np.float32) / np.float32(32))).astype(np.float32)
    ang_r = row * inv_freq
    ang_c = col * inv_freq
    ang = np.concatenate([ang_r, ang_r, ang_c, ang_c], axis=-1)
    cos = np.cos(ang).astype(np.float32)
    sin = np.sin(ang).astype(np.float32)
    d = np.arange(64)
    sign = np.where((d % 32) < 16, -1.0, 1.0).astype(np.float32)
    sins = sin * sign
    cos = np.concatenate([cos, np.ones((256, 64), np.float32)], 0)
    sins = np.concatenate([sins, np.zeros((256, 64), np.float32)], 0)
    cosT = np.ascontiguousarray(np.concatenate([cos.T, cos.T], 0))
    sinT = np.ascontiguousarray(np.concatenate([sins.T, sins.T], 0))
    return cosT, sinT


def perm_blockones():
    import ml_dtypes
    d = np.arange(128)
    sw = np.where((d % 32) < 16, d + 16, d - 16)
    perm = np.zeros((128, 128), np.float32)
    perm[sw, d] = 1.0
    bo = (d[:, None] // 64 == d[None, :] // 64).astype(np.float32)
    return perm.astype(ml_dtypes.bfloat16), bo.astype(ml_dtypes.bfloat16)


def na_bias_tables(rpb, h):
    out = np.full((4, 33, 128, 128), -30000.0, np.float32)
    kk = np.arange(128)
    qq = np.arange(128)
    entries = []
    for s in range(1, 6):
        entries.append((s - 1, 16 * h + 2, s))
    for spec, u in enumerate([0, 1, 14, 15]):
        for s in range(7):
            entries.append((5 + spec * 7 + s, 16 * h + u, s))
    for idx, t, s in entries:
        j = t + s - 3
        kr = (2 * j + kk // 64)[:, None]
        kc = (kk % 64)[:, None]
        r = (2 * t + qq // 64)[None, :]
        qc = (qq % 64)[None, :]
        rs = np.clip(r - 4, 0, 56)
        cs = np.clip(qc - 8, 0, 48)
        valid = (kr >= 0) & (kr <= 63) & (kr >= rs) & (kr <= rs + 7) & (kc >= cs) & (kc <= cs + 15)
        rr = np.clip(kr - r + 7, 0, 14)
        rc = np.clip(kc - qc + 15, 0, 30)
        for hd in range(4):
            g = rpb[hd][rr, rc]
            out[hd, idx] = np.where(valid, g, np.float32(-30000.0))
    return out


def hyena_tables(L):
    import math
    f32 = np.float32
    HY_EMB = 33; HY_BANDS = 16; HY_W = 256
    mn = math.log(1e-2) / 1.5; mx = math.log(1e-2) / 0.3
    t = np.linspace(0.0, 1.0, L, dtype=f32)[:, None]
    w = (f32(2.0 * math.pi / L) * np.arange(L, dtype=f32))[:, None]
    bands = np.linspace(1e-4, HY_BANDS - 1, HY_BANDS, dtype=f32)[None, :]
    z = np.concatenate([t, np.cos(bands * w), -np.sin(bands * w)], axis=-1).astype(f32)
    deltas = np.linspace(mn, mx, HY_W, dtype=f32)
    dec = np.exp(-t * np.abs(deltas)[None, :]).astype(f32)
    ZF = np.ascontiguousarray(z[::-1].T)
    ZB = np.ascontiguousarray(z.T)
    DECF = np.ascontiguousarray(dec[::-1].T)
    DECB = np.ascontiguousarray(dec.T)
    DECB[:, 0] = 0.0
    return ZF, ZB, DECF, DECB


_PROG_CACHE = {}

_IN_SPECS = [
    ("xsrc", [NTOK, D], F32), ("c2T", [128, 8, 2], F32), ("ada_w", [D, 6144], F32), ("ada_b", [6144], F32),
    ("w_in", [D, DIN], F32), ("w_out", [D, D], F32), ("q_gain", [64], F32), ("k_gain", [64], F32),
    ("conv_w", [3, 768], F32), ("conv_b", [768], F32), ("f_w1", [33, 64], F32), ("f_b1", [64], F32), ("f_freq", [2, 64], F32),
    ("f_w2", [64, 64], F32), ("f_b2", [64], F32), ("f_w3", [64, 512], F32), ("f_b3", [512], F32), ("d_skip", [256], F32),
    ("nb_bias", [4, 33, 128, 128], F32), ("ln1_g", [D], F32), ("ln1_b", [D], F32), ("ln2_g", [D], F32), ("ln2_b", [D], F32),
    ("router_w", [D, 32], F32), ("router_b", [32], F32), ("exp_w1", [32, D, 2048], F32), ("exp_b1", [32, 2048], F32),
    ("exp_w2", [32, D, D], F32), ("exp_b2", [32, D], F32),
    ("costab", [128, NTOK], F32), ("sintab", [128, NTOK], F32), ("blockones", [128, 128], BF16), ("perm", [128, 128], BF16),
    ("ZF", [33, 4096], F32), ("ZB", [33, 4096], F32), ("DECF", [256, 4096], F32), ("DECB", [256, 4096], F32),
    ("ZFc", [33, 256], F32), ("ZBc", [33, 256], F32), ("DECFc", [256, 256], F32), ("DECBc", [256, 256], F32),
    ("sel", [128, 2], F32), ("identb", [128, 128], BF16), ("identf", [128, 128], F32), ("revf", [128, 128], F32),
]


def build_layer(need_ctx):
    nc = bass.Bass("TRN2", target_bir_lowering=False)
    NO = NQ if need_ctx else NOWN
    with ExitStack() as es0:
        tr = TR(nc, es0)
        A = {}
        for name, shape, dt in _IN_SPECS:
            A[name] = Buf(nc.dram_tensor(name, shape, dt, kind="ExternalInput").ap(), name)
        XOUT = tr.dram("xout", [NO, D], F32, kind="ExternalOutput")
        modrow = tr.dram("modrow", [2, 6144], F32)
        projT = tr.dram("projT", [DIN, NTOK], F32)
        vtm = tr.dram("vtm", [NTOK, 384], BF16)
        QT = tr.dram("QT", [512, NTOK], BF16)
        KT = tr.dram("KT", [128, NTOK], BF16)
        YT = tr.dram("YT", [1024, NQ], BF16)
        GA = tr.dram("GA", [256, 4096], BF16)
        GB = tr.dram("GB", [256, 4096], BF16)
        GAc = tr.dram("GAc", [256, 512], BF16)
        X1 = tr.dram("X1", [NQ, D], F32)
        with ExitStack() as esp:
            tr.es = esp
            ident_bf = tr.sb([128, 128], BF16, "identb")
            ident_f = tr.sb([128, 128], F32, "identf")
            rev_f = tr.sb([128, 128], F32, "revf")
            tr.dma("sp", ident_bf[:], A["identb"][:], W=[ident_bf])
            tr.dma("sp", ident_f[:], A["identf"][:], W=[ident_f])
            tr.dma("sp", rev_f[:], A["revf"][:], W=[rev_f])
            stage_mod(nc, tr, A["c2T"], A["ada_w"], A["ada_b"], modrow)
            stage_inproj(nc, tr, A["xsrc"], modrow, A["w_in"], projT, vtm, ident_bf)
            stage_qkprep(nc, tr, projT, A["q_gain"], A["k_gain"], A["costab"], A["sintab"], A["blockones"], A["perm"], QT, KT, need_ctx)
            stage_gqa(nc, tr, QT, KT, vtm, YT, need_ctx)
            stage_na(nc, tr, projT, vtm, A["nb_bias"], YT, need_ctx)
            P = {k: A[k] for k in ("conv_w", "conv_b", "f_w1", "f_b1", "f_freq", "f_w2", "f_b2", "f_w3", "f_b3", "d_skip")}
            stage_hyena(nc, tr, projT, P, A, A["sel"], GA, GB, GAc, YT, ident_bf, rev_f, need_ctx)
            stage_outproj(nc, tr, YT, A["w_out"], A["xsrc"], modrow, A["ln1_g"], A["ln1_b"], X1, need_ctx)
            stage_moe(nc, tr, X1, modrow, A["router_w"], A["router_b"], A["exp_w1"], A["exp_b1"], A["exp_w2"], A["exp_b2"],
                      A["ln2_g"], A["ln2_b"], XOUT, ident_f, need_ctx)
            tr.finish()
        mx = max(tr.cnt.values())
        assert mx < 60000, f"semaphore count too large: {mx}"
    return nc


def _const_tables(h):
    import ml_dtypes
    cosT, sinT = rope_tables(h)
    perm, bo = perm_blockones()
    ZF, ZB, DECF, DECB = hyena_tables(4096)
    ZFc, ZBc, DECFc, DECBc = hyena_tables(256)
    eye = np.eye(128, dtype=np.float32)
    return dict(costab=cosT, sintab=sinT, blockones=bo, perm=perm, ZF=ZF, ZB=ZB, DECF=DECF, DECB=DECB,
                ZFc=ZFc, ZBc=ZBc, DECFc=DECFc, DECBc=DECBc,
                sel=np.tile(np.array([[1 - h, h]], np.float32), (128, 1)),
                identb=eye.astype(ml_dtypes.bfloat16), identf=eye, revf=np.ascontiguousarray(eye[::-1]))


def kernel(x, c, ctx, c_ctx, ada_w, ada_b, w_in, w_out, q_gain, k_gain, hy_conv_w, hy_conv_b,
           hy_w1, hy_b1, hy_freq, hy_w2, hy_b2, hy_w3, hy_b3, hy_d, na_rpb, ln1_g, ln1_b,
           ln2_g, ln2_b, router_w, router_b, exp_w1, exp_b1, exp_w2, exp_b2):
    f = lambda a: np.ascontiguousarray(np.asarray(a, dtype=np.float32))
    x = f(x); ctx = f(ctx); c = f(c); c_ctx = f(c_ctx)
    B = x.shape[0]
    consts = [_const_tables(h) for h in range(2)]
    for l in range(2):
        need_ctx = (l == 0)
        if need_ctx not in _PROG_CACHE:
            _PROG_CACHE[need_ctx] = build_layer(need_ctx)
        nc = _PROG_CACHE[need_ctx]
        shared = dict(ada_w=f(ada_w[l]), ada_b=f(ada_b[l]), w_in=f(w_in[l]), w_out=f(w_out[l]), q_gain=f(q_gain[l]), k_gain=f(k_gain[l]),
                      conv_w=f(hy_conv_w[l]), conv_b=f(hy_conv_b[l]), f_w1=f(hy_w1[l]), f_b1=f(hy_b1[l]), f_freq=f(hy_freq[l]),
                      f_w2=f(hy_w2[l]), f_b2=f(hy_b2[l]), f_w3=f(hy_w3[l]), f_b3=f(hy_b3[l]), d_skip=f(hy_d[l]),
                      ln1_g=f(ln1_g[l]), ln1_b=f(ln1_b[l]), ln2_g=f(ln2_g[l]), ln2_b=f(ln2_b[l]),
                      router_w=f(router_w[l]), router_b=f(router_b[l]), exp_w1=f(exp_w1[l]), exp_b1=f(exp_b1[l]),
                      exp_w2=f(exp_w2[l]), exp_b2=f(exp_b2[l]))
        nbb = [na_bias_tables(f(na_rpb[l]), h) for h in range(2)]
        in_maps = []
        for k in range(8):
            b, h = k // 2, k % 2
            ids = local_token_ids(h)
            xs = np.concatenate([x[b][ids], ctx[b]], axis=0)
            c2 = np.stack([c[b], c_ctx], 0)
            c2T = np.ascontiguousarray(c2.reshape(2, 8, 128).transpose(2, 1, 0))
            m = dict(xsrc=np.ascontiguousarray(xs), c2T=c2T, nb_bias=nbb[h])
            m.update(shared)
            m.update(consts[h])
            in_maps.append(m)
        res = run_bass_kernel_spmd(nc, in_maps, core_ids=list(range(8)))
        xn = np.empty_like(x)
        cn = np.empty_like(ctx)
        for k in range(8):
            b, h = k // 2, k % 2
            o = np.asarray(res.results[k]["xout"])
            xn[b, 2048 * h:2048 * (h + 1)] = o[:NOWN]
            if need_ctx and h == 0:
                cn[b] = o[NOWN:]
        x = xn
        if need_ctx:
            ctx = cn
    return x
```

```python
import numpy as np
from contextlib import ExitStack
import concourse.bass as bass
import concourse.mybir as mybir
from concourse.bass_utils import run_bass_kernel_spmd

F32 = mybir.dt.float32
BF16 = mybir.dt.bfloat16
AF = mybir.ActivationFunctionType
ALU = mybir.AluOpType
AX = mybir.AxisListType


class Buf:
    __slots__ = ("t", "w", "r", "name")

    def __init__(self, t, name=""):
        self.t = t
        self.w = None
        self.r = {}
        self.name = name

    def __getitem__(self, k):
        return self.t[k]


class TR:
    NDMA = 24

    def __init__(self, nc, es, tag=""):
        self.nc = nc
        self.es = es
        self.eng = {"pe": nc.tensor, "act": nc.scalar, "dve": nc.vector, "pool": nc.gpsimd, "sp": nc.sync}
        self.sem = {}
        self.cnt = {}
        for k in ("pe", "act", "dve", "pool"):
            self.sem[k] = es.enter_context(nc.semaphore(f"{tag}s_{k}"))
            self.cnt[k] = 0
        for i in range(self.NDMA):
            k = f"d{i}"
            self.sem[k] = es.enter_context(nc.semaphore(f"{tag}s_{k}"))
            self.cnt[k] = 0
        self.waited = {}
        self.dma_rr = {"hw": 0, "sw": 0}
        self.NHW = 16
        self.pending = {k: False for k in ("pe", "act", "dve", "pool")}
        self.nbuf = 0

    def sb(self, shape, dtype=F32, name=None):
        self.nbuf += 1
        name = name or f"sb{self.nbuf}"
        t = self.es.enter_context(self.nc.sbuf_tensor(f"{name}_{self.nbuf}", list(shape), dtype))
        return Buf(t, name)

    def ps(self, shape, dtype=F32, name=None):
        self.nbuf += 1
        name = name or f"ps{self.nbuf}"
        t = self.es.enter_context(self.nc.psum_tensor(f"{name}_{self.nbuf}", list(shape), dtype))
        return Buf(t, name)

    def dram(self, name, shape, dtype=F32, kind="Internal"):
        t = self.nc.dram_tensor(name, list(shape), dtype, kind=kind)
        return Buf(t.ap(), name)

    def _deps(self, R, W):
        deps = {}
        for b in R:
            if b.w is not None:
                k, v = b.w
                deps[k] = max(deps.get(k, 0), v)
        for b in W:
            if b.w is not None:
                k, v = b.w
                deps[k] = max(deps.get(k, 0), v)
            for k, v in b.r.items():
                deps[k] = max(deps.get(k, 0), v)
        return deps

    def _wait(self, e, deps, skip_self=False):
        engine = self.eng[e]
        for k, v in deps.items():
            if skip_self and k == e:
                continue
            if self.waited.get((e, k), 0) >= v:
                continue
            engine.wait_ge(self.sem[k], v)
            self.waited[(e, k)] = v

    def _mark(self, ev, R, W):
        k, v = ev
        for b in W:
            b.w = ev
            b.r = {}
        for b in R:
            if b.r.get(k, 0) < v:
                b.r[k] = v

    def op(self, e, fn, R=(), W=(), sig=True):
        deps = self._deps(R, W)
        self._wait(e, deps, skip_self=(e == "pe"))
        ins = fn(self.eng[e])
        if sig:
            self.cnt[e] += 1
            ins.then_inc(self.sem[e], 1)
            ev = (e, self.cnt[e])
        else:
            ev = (e, self.cnt[e] + 1)
        self._mark(ev, R, W)
        return ins

    def dma(self, q, out, in_, R=(), W=(), **kw):
        deps = self._deps(R, W)
        self._wait(q, deps)
        if q == "pool":
            slot = f"d{self.NHW + self.dma_rr['sw']}"
            self.dma_rr["sw"] = (self.dma_rr["sw"] + 1) % (self.NDMA - self.NHW)
        else:
            slot = f"d{self.dma_rr['hw']}"
            self.dma_rr["hw"] = (self.dma_rr["hw"] + 1) % self.NHW
        prev = self.cnt[slot]
        if prev > 0 and self.waited.get((q, slot), 0) < prev:
            self.eng[q].wait_ge(self.sem[slot], prev)
            self.waited[(q, slot)] = prev
        ins = self.eng[q].dma_start(out=out, in_=in_, **kw)
        self.cnt[slot] = prev + 16
        ins.then_inc(self.sem[slot], 16)
        self._mark((slot, prev + 16), R, W)
        return ins

    def barrier(self):
        for e in ("pe", "act", "dve", "pool", "sp"):
            self._wait(e, dict((k, v) for k, v in self.cnt.items() if v > 0))

    def finish(self):
        sp = self.nc.sync
        for k, v in self.cnt.items():
            if v > 0:
                sp.wait_ge(self.sem[k], v)


def interleave(gens, width=2):
    gens = list(gens)
    active = []
    nxt = 0
    while active or nxt < len(gens):
        while len(active) < width and nxt < len(gens):
            active.append(gens[nxt])
            nxt += 1
        for g in list(active):
            try:
                next(g)
            except StopIteration:
                active.remove(g)


D = 1024
DIN = 2304
NLAT = 4096
NCTX = 256
NTOK = NLAT + NCTX
EPS = 1e-6


def bcast_rows(ap_row, nparts=128):
    return ap_row.partition_broadcast(nparts)


def stage_mod(nc, tr, c2T, ada_w, ada_b, modrow):
    with ExitStack() as es:
        tr.es = es
        cs = tr.sb([128, 8, 2], F32, "cs")
        sc = tr.sb([128, 8, 2], F32, "sc")
        sg = tr.sb([128, 8, 2], F32, "sg")
        tr.dma("sp", cs[:], c2T[:], R=[c2T], W=[cs])
        tr.op("act", lambda e: e.activation(out=sg[:], in_=cs[:], func=AF.Sigmoid), R=[cs], W=[sg])
        tr.op("dve", lambda e: e.tensor_mul(out=sc[:], in0=cs[:], in1=sg[:]), R=[cs, sg], W=[sc])
        bias = tr.sb([2, 6144], F32, "bias")
        tr.dma("sp", bias[:], ada_b[:].partition_broadcast(2), R=[ada_b], W=[bias])
        res = tr.sb([2, 6144], F32, "res")
        wbufs = [tr.sb([128, 8, 512], F32, f"aw{i}") for i in range(2)]
        pss = [tr.ps([128, 512], F32, f"pm{i}") for i in range(2)]
        for j in range(12):
            wb = wbufs[j % 2]
            ps = pss[j % 2]
            tr.dma("sp", wb[:], ada_w[:, j * 512:(j + 1) * 512].rearrange("(k p) n -> p k n", p=128), R=[ada_w], W=[wb])
            for k in range(8):
                tr.op("pe", lambda e, k=k: e.matmul(ps[0:2, :], lhsT=sc[:, k, :], rhs=wb[:, k, :], start=(k == 0), stop=(k == 7)),
                      R=[sc, wb], W=[ps], sig=(k == 7))
            tr.op("dve", lambda e: e.tensor_add(out=res[:, j * 512:(j + 1) * 512], in0=ps[0:2, :], in1=bias[:, j * 512:(j + 1) * 512]),
                  R=[ps, bias], W=[res])
        tr.dma("sp", modrow[:], res[:], R=[res], W=[modrow])
        tr.barrier()


def load_modT(nc, tr, modrow, r, names):
    t = tr.sb([128, 48], F32, "modT")
    with nc.allow_non_contiguous_dma(reason="tiny"):
        tr.dma("sp", t[:], modrow[r, :].rearrange("(j p) -> p j", p=128), R=[modrow], W=[t])
    return t


def ln_stats(tr, xt, stats, mv, rstd, tmp):
    for hseg in range(2):
        tr.op("dve", lambda e, hseg=hseg: e.bn_stats(out=stats[:, hseg * 6:(hseg + 1) * 6], in_=xt[:, hseg * 512:(hseg + 1) * 512]),
              R=[xt], W=[stats])
    tr.op("dve", lambda e: e.bn_aggr(out=mv[:], in_=stats[:]), R=[stats], W=[mv])
    tr.op("act", lambda e: e.activation(out=tmp[:], in_=mv[:, 1:2], func=AF.Sqrt, bias=EPS), R=[mv], W=[tmp])
    tr.op("dve", lambda e: e.reciprocal(out=rstd[:], in_=tmp[:]), R=[tmp], W=[rstd])


def stage_inproj(nc, tr, xsrc, modrow, w_in, projT, vtm, ident_bf):
    with ExitStack() as es:
        tr.es = es
        w = tr.sb([128, 8, DIN], BF16, "w_in")
        for k in range(8):
            tr.dma("pool", w[:, k, :], w_in[k * 128:(k + 1) * 128, :], R=[w_in], W=[w])
        mods = []
        for r in range(2):
            mT = load_modT(nc, tr, modrow, r, None)
            sp1 = tr.sb([128, 8], F32, "sp1")
            tr.op("dve", lambda e, mT=mT, sp1=sp1: e.tensor_scalar_add(out=sp1[:], in0=mT[:, 8:16], scalar1=1.0), R=[mT], W=[sp1])
            mods.append((mT, sp1))
        NB = NTOK // 512 + (1 if NTOK % 512 else 0)
        xts = [tr.sb([128, D], F32, f"xt{i}") for i in range(3)]
        xns = [tr.sb([128, D], BF16, f"xn{i}") for i in range(2)]
        hTs = [tr.sb([128, 8, 512], BF16, f"hT{i}") for i in range(2)]
        lnb_ = [[tr.sb([128, 12], F32, f"stats{i}"), tr.sb([128, 2], F32, f"mv{i}"), tr.sb([128, 1], F32, f"rstd{i}"),
                 tr.sb([128, 1], F32, f"tmp{i}"), tr.sb([128, 1], F32, f"nmr{i}")] for i in range(3)]
        pT = [tr.ps([128, 8, 128], BF16, f"pT{i}") for i in range(2)]
        pO = [tr.ps([128, 512], F32, f"pO{i}") for i in range(4)]
        osb = [tr.sb([128, 512], F32, f"osb{i}") for i in range(4)]
        vsb = [tr.sb([128, 384], BF16, f"vsb{i}") for i in range(2)]
        st = {"it": 0, "oi": 0}

        def phaseA(blk):
            t0 = blk * 512
            nt = min(512, NTOK - t0)
            ntile = nt // 128
            hT = hTs[blk % 2]
            r = 1 if t0 >= NLAT else 0
            mT, sp1 = mods[r]
            for ti in range(ntile):
                it = st["it"]
                xt = xts[it % 3]
                xn = xns[it % 2]
                p = pT[it % 2]
                stats, mv, rstd, tmp, nmr = lnb_[it % 3]
                st["it"] += 1
                tok = t0 + ti * 128
                tr.dma("sp", xt[:], xsrc[tok:tok + 128, :], R=[xsrc], W=[xt])
                ln_stats(tr, xt, stats, mv, rstd, tmp)
                tr.op("dve", lambda e: e.scalar_tensor_tensor(out=nmr[:], in0=mv[:, 0:1], scalar=-1.0, in1=rstd[:], op0=ALU.mult, op1=ALU.mult),
                      R=[mv, rstd], W=[nmr])
                tr.op("act", lambda e, xt=xt, xn=xn: e.activation(out=xn[:], in_=xt[:], func=AF.Identity, scale=rstd[:], bias=nmr[:]),
                      R=[xt, rstd, nmr], W=[xn])
                yield
                for c in range(8):
                    tr.op("pe", lambda e, c=c, xn=xn, p=p: e.transpose(out=p[:, c, :], in_=xn[:, c * 128:(c + 1) * 128], identity=ident_bf[:]),
                          R=[xn, ident_bf], W=[p], sig=(c == 7))
                for c in range(8):
                    tr.op("dve" if c % 2 else "act",
                          (lambda e, c=c, p=p: e.tensor_scalar(out=hT[:, c, ti * 128:(ti + 1) * 128], in0=p[:, c, :], scalar1=sp1[:, c:c + 1],
                                                               scalar2=mT[:, c:c + 1], op0=ALU.mult, op1=ALU.add)) if c % 2 else
                          (lambda e, c=c, p=p: e.activation(out=hT[:, c, ti * 128:(ti + 1) * 128], in_=p[:, c, :], func=AF.Identity,
                                                            scale=sp1[:, c:c + 1], bias=mT[:, c:c + 1])),
                          R=[p, sp1, mT], W=[hT])
                yield

        def phaseB(blk):
            t0 = blk * 512
            nt = min(512, NTOK - t0)
            ntile = nt // 128
            hT = hTs[blk % 2]
            for cc in range(DIN // 128):
                oi = st["oi"]
                po = pO[oi % 4]
                ob = osb[oi % 4]
                st["oi"] += 1
                for k in range(8):
                    tr.op("pe", lambda e, k=k, cc=cc, po=po: e.matmul(po[:, 0:nt], lhsT=w[:, k, cc * 128:(cc + 1) * 128], rhs=hT[:, k, 0:nt],
                                                                      start=(k == 0), stop=(k == 7)), R=[w, hT], W=[po], sig=(k == 7))
                tr.op("dve" if cc % 2 else "act",
                      (lambda e, po=po, ob=ob: e.tensor_copy(out=ob[:, 0:nt], in_=po[:, 0:nt])) if cc % 2 else
                      (lambda e, po=po, ob=ob: e.activation(out=ob[:, 0:nt], in_=po[:, 0:nt], func=AF.Copy)),
                      R=[po], W=[ob])
                tr.dma("sp", projT[cc * 128:(cc + 1) * 128, t0:t0 + nt], ob[:, 0:nt], R=[ob], W=[projT])
                yield
            for ti in range(ntile):
                oi = st["oi"]
                po = pO[oi % 4]
                vb = vsb[oi % 2]
                st["oi"] += 1
                for k in range(8):
                    tr.op("pe", lambda e, k=k, po=po: e.matmul(po[:, 0:128], lhsT=hT[:, k, ti * 128:(ti + 1) * 128], rhs=w[:, k, 640:768],
                                                               start=(k == 0), stop=(k == 7)), R=[w, hT], W=[po], sig=False)
                for k in range(8):
                    tr.op("pe", lambda e, k=k, po=po: e.matmul(po[:, 128:384], lhsT=hT[:, k, ti * 128:(ti + 1) * 128], rhs=w[:, k, 2048:2304],
                                                               start=(k == 0), stop=(k == 7)), R=[w, hT], W=[po], sig=(k == 7))
                tr.op("dve", lambda e, po=po, vb=vb: e.tensor_copy(out=vb[:], in_=po[:, 0:384]), R=[po], W=[vb])
                tok = t0 + ti * 128
                tr.dma("sp", vtm[tok:tok + 128, :], vb[:], R=[vb], W=[vtm])
                yield

        for _ in phaseA(0):
            pass
        for blk in range(NB):
            gB = phaseB(blk)
            gA = phaseA(blk + 1) if blk + 1 < NB else None
            doneA = gA is None
            doneB = False
            while not (doneA and doneB):
                for _ in range(3):
                    if not doneB:
                        try:
                            next(gB)
                        except StopIteration:
                            doneB = True
                if not doneA:
                    try:
                        next(gA)
                    except StopIteration:
                        doneA = True
        tr.barrier()


NOWN = 2048
NQ = NOWN + NCTX
NKC = NTOK // 128


def stage_qkprep(nc, tr, projT, q_gain, k_gain, costab, sintab, blockones, perm, QT, KT, need_ctx):
    with ExitStack() as es:
        tr.es = es
        bo = tr.sb([128, 128], BF16, "bo")
        pm = tr.sb([128, 128], BF16, "pm")
        tr.dma("sp", bo[:], blockones[:], R=[blockones], W=[bo])
        tr.dma("sp", pm[:], perm[:], R=[perm], W=[pm])
        gq = tr.sb([128, 1], F32, "gq")
        gk = tr.sb([128, 1], F32, "gk")
        for g, src in ((gq, q_gain), (gk, k_gain)):
            for half in range(2):
                tr.dma("sp", g[half * 64:(half + 1) * 64, :], src[:].rearrange("(p o) -> p o", o=1), R=[src], W=[g])
        R3 = 3
        xs = [tr.sb([128, 512], F32, f"x{i}") for i in range(R3)]
        cs = [tr.sb([128, 512], F32, f"c{i}") for i in range(R3)]
        ss_ = [tr.sb([128, 512], F32, f"s{i}") for i in range(R3)]
        sq = [tr.sb([128, 512], BF16, f"sq{i}") for i in range(2)]
        sd = [tr.sb([128, 512], F32, f"sd{i}") for i in range(2)]
        ri = [tr.sb([128, 512], F32, f"ri{i}") for i in range(2)]
        yb = [tr.sb([128, 512], BF16, f"yb{i}") for i in range(2)]
        t1 = [tr.sb([128, 512], F32, f"t1{i}") for i in range(2)]
        t2 = [tr.sb([128, 512], F32, f"t2{i}") for i in range(2)]
        ob = [tr.sb([128, 512], BF16, f"ob{i}") for i in range(2)]
        pA = [tr.ps([128, 512], F32, f"pA{i}") for i in range(2)]
        pB = [tr.ps([128, 512], F32, f"pB{i}") for i in range(2)]
        work = []
        own_blocks = [(i * 512, 512) for i in range(4)]
        oth_blocks = [(NOWN + i * 512, 512) for i in range(4)]
        ctx_block = [(NLAT, NCTX)]
        for rg in range(4):
            for (t0, nt) in own_blocks + (ctx_block if need_ctx else []):
                work.append((rg * 128, QT, rg * 128, gq, t0, nt))
        for (t0, nt) in own_blocks + oth_blocks + ctx_block:
            work.append((512, KT, 0, gk, t0, nt))
        for i, (srow, dst, drow, g, t0, nt) in enumerate(work):
            x = xs[i % R3]; c = cs[i % R3]; s = ss_[i % R3]
            j = i % 2
            tr.dma("sp", x[:, 0:nt], projT[srow:srow + 128, t0:t0 + nt], R=[projT], W=[x])
            tr.dma("sp", c[:, 0:nt], costab[:, t0:t0 + nt], R=[costab], W=[c])
            tr.dma("sp", s[:, 0:nt], sintab[:, t0:t0 + nt], R=[sintab], W=[s])
            tr.op("act", lambda e: e.activation(out=sq[j][:, 0:nt], in_=x[:, 0:nt], func=AF.Square), R=[x], W=[sq[j]])
            tr.op("pe", lambda e: e.matmul(pA[j][:, 0:nt], lhsT=bo[:], rhs=sq[j][:, 0:nt], start=True, stop=True), R=[bo, sq[j]], W=[pA[j]])
            tr.op("act", lambda e: e.activation(out=sd[j][:, 0:nt], in_=pA[j][:, 0:nt], func=AF.Sqrt, scale=1.0 / 64, bias=EPS), R=[pA[j]], W=[sd[j]])
            tr.op("dve", lambda e: e.reciprocal(out=ri[j][:, 0:nt], in_=sd[j][:, 0:nt]), R=[sd[j]], W=[ri[j]])
            tr.op("dve", lambda e: e.scalar_tensor_tensor(out=yb[j][:, 0:nt], in0=x[:, 0:nt], scalar=g[:, 0:1], in1=ri[j][:, 0:nt], op0=ALU.mult, op1=ALU.mult),
                  R=[x, g, ri[j]], W=[yb[j]])
            tr.op("pe", lambda e: e.matmul(pB[j][:, 0:nt], lhsT=pm[:], rhs=yb[j][:, 0:nt], start=True, stop=True), R=[pm, yb[j]], W=[pB[j]])
            tr.op("pool", lambda e: e.tensor_mul(out=t1[j][:, 0:nt], in0=yb[j][:, 0:nt], in1=c[:, 0:nt]), R=[yb[j], c], W=[t1[j]])
            tr.op("dve", lambda e: e.tensor_mul(out=t2[j][:, 0:nt], in0=pB[j][:, 0:nt], in1=s[:, 0:nt]), R=[pB[j], s], W=[t2[j]])
            tr.op("dve", lambda e: e.tensor_add(out=ob[j][:, 0:nt], in0=t1[j][:, 0:nt], in1=t2[j][:, 0:nt]), R=[t1[j], t2[j]], W=[ob[j]])
            tr.dma("sp", dst[drow:drow + 128, t0:t0 + nt], ob[j][:, 0:nt], R=[ob[j]], W=[dst])
        tr.barrier()


class AttnCtx:
    def __init__(self, tr):
        self.pS = [tr.ps([128, 512], F32, f"pS{i}") for i in range(4)]
        self.pO = [tr.ps([64, 512], F32, f"pOa{i}") for i in range(2)]
        self.pZ = [tr.ps([64, 512], F32, f"pZ{i}") for i in range(2)]
        self.PT = [tr.sb([128, 512], BF16, f"PT{i}") for i in range(4)]
        self.rinv = [tr.sb([64, 512], F32, f"rinv{i}") for i in range(2)]
        self.yo = [tr.sb([64, 512], BF16, f"yo{i}") for i in range(2)]
        self.ones = tr.sb([128, 64], BF16, "ones")
        tr.op("dve", lambda e: e.memset(self.ones[:], 1.0), W=[self.ones])
        self.i = 0
        self.o = 0


def attn_block(tr, ac, *a, **kw):
    for _ in attn_block_gen(tr, ac, *a, **kw):
        pass


def attn_block_gen(tr, ac, Qh, q0, nq, Kg, V, vcol, chunks, YT, yrow, ycol, mask_fn=None, pre=None):
    if pre is not None:
        pre()
    o = ac.o % 2
    ac.o += 1
    pO, pZ = ac.pO[o], ac.pZ[o]
    n = len(chunks)
    slots = []

    def issue_qk(ci):
        i = ac.i % 4
        ac.i += 1
        pS, PT = ac.pS[i], ac.PT[i]
        ch = chunks[ci]
        tr.op("pe", lambda e: e.matmul(pS[:, 0:nq], lhsT=Kg[:, ch * 128:(ch + 1) * 128], rhs=Qh[:, q0:q0 + nq], start=True, stop=True),
              R=[Kg, Qh], W=[pS])
        slots.append((pS, PT))

    LOOK = 3
    for ci in range(min(LOOK, n)):
        issue_qk(ci)
    for ci, ch in enumerate(chunks):
        pS, PT = slots[ci]
        tr.op("act", lambda e: e.activation(out=PT[:, 0:nq], in_=pS[:, 0:nq], func=AF.Exp, scale=0.125), R=[pS], W=[PT])
        if mask_fn is not None:
            m = mask_fn(ci)
            if m is not None:
                mb, map_ = m
                tr.op("dve", lambda e: e.tensor_mul(out=PT[:, 0:nq], in0=PT[:, 0:nq], in1=map_), R=[PT, mb], W=[PT])
        if ci + LOOK < n:
            issue_qk(ci + LOOK)
        tr.op("pe", lambda e: e.matmul(pO[:, 0:nq], lhsT=V[:, ch, vcol:vcol + 64], rhs=PT[:, 0:nq], start=(ci == 0), stop=(ci == n - 1)),
              R=[V, PT], W=[pO], sig=False)
        tr.op("pe", lambda e: e.matmul(pZ[:, 0:nq], lhsT=ac.ones[:], rhs=PT[:, 0:nq], start=(ci == 0), stop=(ci == n - 1)),
              R=[ac.ones, PT], W=[pZ, pO], sig=True)
        yield
    rinv, yo = ac.rinv[o], ac.yo[o]
    tr.op("dve", lambda e: e.reciprocal(out=rinv[:, 0:nq], in_=pZ[:, 0:nq]), R=[pZ], W=[rinv])
    tr.op("dve", lambda e: e.tensor_mul(out=yo[:, 0:nq], in0=pO[:, 0:nq], in1=rinv[:, 0:nq]), R=[pO, rinv], W=[yo])
    tr.dma("sp", YT[yrow:yrow + 64, ycol:ycol + nq], yo[:, 0:nq], R=[yo], W=[YT])


def stage_gqa(nc, tr, QT, KT, vtm, YT, need_ctx):
    with ExitStack() as es:
        tr.es = es
        ac = AttnCtx(tr)
        V = tr.sb([128, NKC, 128], BF16, "V")
        tr.dma("sp", V[:], vtm[:, 0:128].rearrange("(c p) n -> p c n", p=128), R=[vtm], W=[V])
        Kg = [tr.sb([64, NTOK], BF16, f"K{g}") for g in range(2)]
        for g in range(2):
            tr.dma("sp", Kg[g][:], KT[g * 64:(g + 1) * 64, :], R=[KT], W=[Kg[g]])
        Qh = [tr.sb([64, NOWN + NCTX], BF16, f"Q{i}") for i in range(2)]
        allchunks = list(range(NKC))
        ctxchunks = [32, 33]
        gens = []
        for hq in range(8):
            q = Qh[hq % 2]
            g = hq // 4

            def pre(q=q, hq=hq):
                tr.dma("sp", q[:, 0:NOWN], QT[hq * 64:(hq + 1) * 64, 0:NOWN], R=[QT], W=[q])
                if need_ctx:
                    tr.dma("sp", q[:, NOWN:NOWN + NCTX], QT[hq * 64:(hq + 1) * 64, NLAT:NLAT + NCTX], R=[QT], W=[q])
            for qb in range(4):
                gens.append(attn_block_gen(tr, ac, q, qb * 512, 512, Kg[g], V, g * 64, allchunks, YT, hq * 64, qb * 512, pre=(pre if qb == 0 else None)))
            if need_ctx:
                gens.append(attn_block_gen(tr, ac, q, NOWN, NCTX, Kg[g], V, g * 64, ctxchunks, YT, hq * 64, NOWN))
        interleave(gens, width=1)
        tr.barrier()


def na_slots(u):
    if 2 <= u <= 13:
        return [(s, s - 1) for s in range(1, 6)]
    spec = {0: 0, 1: 1, 14: 2, 15: 3}[u]
    return [(s, 5 + spec * 7 + s) for s in range(7)]


def stage_na(nc, tr, projT, vtm, nb_bias, YT, need_ctx):
    with ExitStack() as es:
        tr.es = es
        ac = AttnCtx(tr)
        V = tr.sb([128, NKC, 256], BF16, "NV")
        tr.dma("sp", V[:], vtm[:, 128:384].rearrange("(c p) n -> p c n", p=128), R=[vtm], W=[V])
        EB = tr.sb([128, 4, 33, 128], BF16, "EB")
        stg = [tr.sb([128, 11, 128], F32, f"stg{i}") for i in range(2)]
        k = 0
        for h in range(4):
            for part in range(3):
                st = stg[k % 2]
                k += 1
                tr.dma("sp", st[:], nb_bias[h, part * 11:(part + 1) * 11, :, :].rearrange("s k q -> k s q"), R=[nb_bias], W=[st])
                tr.op("act", lambda e: e.activation(out=EB[:, h, part * 11:(part + 1) * 11, :], in_=st[:], func=AF.Exp), R=[st], W=[EB])
        Kh = [tr.sb([64, NTOK], BF16, f"NK{i}") for i in range(2)]
        Qh = [tr.sb([64, NOWN + NCTX], BF16, f"NQ{i}") for i in range(2)]
        gens = []
        for h in range(4):
            kk = Kh[h % 2]; q = Qh[h % 2]

            def pre(kk=kk, q=q, h=h):
                tr.dma("pool", kk[:], projT[1792 + h * 64:1792 + (h + 1) * 64, :], R=[projT], W=[kk])
                tr.dma("pool", q[:, 0:NOWN], projT[1536 + h * 64:1536 + (h + 1) * 64, 0:NOWN], R=[projT], W=[q])
                if need_ctx:
                    tr.dma("pool", q[:, NOWN:NOWN + NCTX], projT[1536 + h * 64:1536 + (h + 1) * 64, NLAT:NLAT + NCTX], R=[projT], W=[q])
            for u in range(16):
                slots = na_slots(u)
                chunks = [32, 33] + [(u + s - 3) % 32 for s, _ in slots]

                def mask_fn(ci, slots=slots, h=h):
                    if ci < 2:
                        return None
                    return EB, EB[:, h, slots[ci - 2][1], :]
                gens.append(attn_block_gen(tr, ac, q, u * 128, 128, kk, V, h * 64, chunks, YT, 768 + h * 64, u * 128, mask_fn=mask_fn, pre=(pre if u == 0 else None)))
            if need_ctx:
                gens.append(attn_block_gen(tr, ac, q, NOWN, NCTX, kk, V, h * 64, [32, 33], YT, 768 + h * 64, NOWN))
        interleave(gens, width=1)
        tr.barrier()

import math

PI = math.pi


def _wrap(tr, a, t, n):
    for _ in range(2):
        tr.op("dve", lambda e: e.tensor_scalar(out=t[:, 0:n], in0=a[:, 0:n], scalar1=PI, scalar2=-2 * PI, op0=ALU.is_gt, op1=ALU.mult), R=[a], W=[t])
        tr.op("dve", lambda e: e.tensor_add(out=a[:, 0:n], in0=a[:, 0:n], in1=t[:, 0:n]), R=[a, t], W=[a])
        tr.op("dve", lambda e: e.tensor_scalar(out=t[:, 0:n], in0=a[:, 0:n], scalar1=-PI, scalar2=2 * PI, op0=ALU.is_lt, op1=ALU.mult), R=[a], W=[t])
        tr.op("dve", lambda e: e.tensor_add(out=a[:, 0:n], in0=a[:, 0:n], in1=t[:, 0:n]), R=[a, t], W=[a])


def hyena_filter(nc, tr, P, ZF, ZB, DECF, DECB, L, nlag, GA, GB, sel):
    with ExitStack() as es:
        tr.es = es
        w1 = tr.sb([33, 64], F32, "w1"); w2 = tr.sb([64, 64], F32, "w2"); w3 = tr.sb([64, 512], F32, "w3")
        tr.dma("sp", w1[:], P["f_w1"][:], R=[P["f_w1"]], W=[w1])
        tr.dma("sp", w2[:], P["f_w2"][:], R=[P["f_w2"]], W=[w2])
        tr.dma("sp", w3[:], P["f_w3"][:], R=[P["f_w3"]], W=[w3])
        col = lambda ap: ap.rearrange("(p o) -> p o", o=1)
        fr = tr.sb([64, 2], F32, "fr"); bb = tr.sb([64, 2], F32, "bb"); fb = tr.sb([64, 2], F32, "fb")
        for i in range(2):
            tr.dma("sp", fr[:, i:i + 1], col(P["f_freq"][i, :]), R=[P["f_freq"]], W=[fr])
        tr.dma("sp", bb[:, 0:1], col(P["f_b1"][:]), R=[P["f_b1"]], W=[bb])
        tr.dma("sp", bb[:, 1:2], col(P["f_b2"][:]), R=[P["f_b2"]], W=[bb])
        tr.op("dve", lambda e: e.tensor_mul(out=fb[:], in0=fr[:], in1=bb[:]), R=[fr, bb], W=[fb])
        b3 = tr.sb([128, 4], F32, "b3")
        for i in range(4):
            tr.dma("sp", b3[:, i:i + 1], col(P["f_b3"][i * 128:(i + 1) * 128]), R=[P["f_b3"]], W=[b3])
        Z = [tr.sb([33, L], F32, "ZF"), tr.sb([33, L], F32, "ZB")]
        tr.dma("sp", Z[0][:], ZF[:], R=[ZF], W=[Z[0]])
        tr.dma("sp", Z[1][:], ZB[:], R=[ZB], W=[Z[1]])
        H = [[tr.sb([128, L], F32, f"H{d}{c}") for c in range(2)] for d in range(2)]
        BLK = min(512, L)
        fbuf = [dict(a=[tr.sb([64, BLK], F32, f"a{i}{r}") for i in range(2)], t=[tr.sb([64, BLK], F32, f"t{i}{r}") for i in range(2)],
                     h1=tr.sb([64, BLK], F32, f"h1{r}"), h2=tr.sb([64, BLK], F32, f"h2{r}"),
                     dec=[tr.sb([128, BLK], F32, f"dec{i}{r}") for i in range(2)],
                     p1=tr.ps([64, BLK], F32, f"p1{r}"), p2=tr.ps([64, BLK], F32, f"p2{r}"),
                     p3=[tr.ps([128, BLK], F32, f"p3{i}{r}") for i in range(2)]) for r in range(2)]
        DEC = [DECF, DECB]
        bi = 0
        for d in range(2):
            for b in range(L // BLK):
                sl = slice(b * BLK, (b + 1) * BLK)
                fb_ = fbuf[bi % 2]
                bi += 1
                a = fb_["a"]; h1 = fb_["h1"]; h2 = fb_["h2"]; dec = fb_["dec"]; p1 = fb_["p1"]; p2 = fb_["p2"]; p3 = fb_["p3"]
                tr.op("pe", lambda e: e.matmul(p1[:], lhsT=w1[:], rhs=Z[d][:, sl], start=True, stop=True), R=[w1, Z[d]], W=[p1])
                tr.op("dve", lambda e: e.tensor_scalar(out=a[0][:], in0=p1[:], scalar1=fr[:, 0:1], scalar2=fb[:, 0:1], op0=ALU.mult, op1=ALU.add), R=[p1, fr, fb], W=[a[0]])
                _wrap(tr, a[0], fb_['t'][0], BLK)
                tr.op("act", lambda e: e.activation(out=h1[:], in_=a[0][:], func=AF.Sin), R=[a[0]], W=[h1])
                tr.op("pe", lambda e: e.matmul(p2[:], lhsT=w2[:], rhs=h1[:], start=True, stop=True), R=[w2, h1], W=[p2])
                tr.op("dve", lambda e: e.tensor_scalar(out=a[1][:], in0=p2[:], scalar1=fr[:, 1:2], scalar2=fb[:, 1:2], op0=ALU.mult, op1=ALU.add), R=[p2, fr, fb], W=[a[1]])
                _wrap(tr, a[1], fb_['t'][1], BLK)
                tr.op("act", lambda e: e.activation(out=h2[:], in_=a[1][:], func=AF.Sin), R=[a[1]], W=[h2])
                for c in range(2):
                    tr.dma("sp", dec[c][:], DEC[d][c * 128:(c + 1) * 128, sl], R=[DEC[d]], W=[dec[c]])
                    tr.op("pe", lambda e: e.matmul(p3[c][:], lhsT=w3[:, d * 256 + c * 128:d * 256 + (c + 1) * 128], rhs=h2[:], start=True, stop=True), R=[w3, h2], W=[p3[c]])
                    tr.op("dve", lambda e: e.scalar_tensor_tensor(out=H[d][c][:, sl], in0=p3[c][:], scalar=b3[:, d * 2 + c:d * 2 + c + 1], in1=dec[c][:], op0=ALU.add, op1=ALU.mult),
                          R=[p3[c], b3, dec[c]], W=[H[d][c]])
        junk = tr.sb([128, L], BF16, "junk")
        ss = tr.sb([128, 4], F32, "ss")
        tot = tr.sb([128, 2], F32, "tot"); sd = tr.sb([128, 2], F32, "sd"); nrm = tr.sb([128, 2], F32, "nrm")
        n0 = tr.sb([128, 2], F32, "n0"); n1 = tr.sb([128, 2], F32, "n1")
        selt = tr.sb([128, 2], F32, "selt")
        tr.dma("sp", selt[:], sel[:], R=[sel], W=[selt])
        for d in range(2):
            for c in range(2):
                tr.op("act", lambda e: e.activation(out=junk[:], in_=H[d][c][:], func=AF.Square, accum_out=ss[:, d * 2 + c:d * 2 + c + 1]), R=[H[d][c]], W=[junk, ss])
        tr.op("dve", lambda e: e.tensor_add(out=tot[:], in0=ss[:, 0:2], in1=ss[:, 2:4]), R=[ss], W=[tot])
        tr.op("act", lambda e: e.activation(out=sd[:], in_=tot[:], func=AF.Sqrt, bias=EPS), R=[tot], W=[sd])
        tr.op("dve", lambda e: e.reciprocal(out=nrm[:], in_=sd[:]), R=[sd], W=[nrm])
        tr.op("dve", lambda e: e.tensor_scalar_mul(out=n0[:], in0=nrm[:], scalar1=selt[:, 0:1]), R=[nrm, selt], W=[n0])
        tr.op("dve", lambda e: e.tensor_scalar_mul(out=n1[:], in0=nrm[:], scalar1=selt[:, 1:2]), R=[nrm, selt], W=[n1])
        WA = GA.t.shape[1]
        for c in range(2):
            ga = tr.sb([128, WA], BF16, f"ga{c}")
            tr.op("pool", lambda e: e.memset(ga[:], 0.0), W=[ga])
            tr.op("dve", lambda e: e.tensor_scalar_mul(out=ga[:, 0:nlag + 1], in0=H[0][c][:, L - 1 - nlag:L], scalar1=nrm[:, c:c + 1]), R=[H[0][c], nrm], W=[ga])
            tr.op("dve", lambda e: e.tensor_scalar_mul(out=ga[:, nlag + 1:2 * nlag + 1], in0=H[1][c][:, 1:nlag + 1], scalar1=nrm[:, c:c + 1]), R=[H[1][c], nrm], W=[ga])
            tr.dma("sp", GA[c * 128:(c + 1) * 128, :], ga[:], R=[ga], W=[GA])
            if GB is not None:
                tmp = tr.sb([128, L], F32, f"gtmp{c}")
                gb = tr.sb([128, L], BF16, f"gb{c}")
                tr.op("pool", lambda e: e.memset(gb[:], 0.0), W=[gb])
                tr.op("dve", lambda e: e.tensor_scalar_mul(out=tmp[:, 0:L - 1], in0=H[1][c][:, 1:L], scalar1=n0[:, c:c + 1]), R=[H[1][c], n0], W=[tmp])
                tr.op("dve", lambda e: e.scalar_tensor_tensor(out=gb[:, 0:L - 1], in0=H[0][c][:, 0:L - 1], scalar=n1[:, c:c + 1], in1=tmp[:, 0:L - 1], op0=ALU.mult, op1=ALU.add),
                      R=[H[0][c], n1, tmp], W=[gb])
                tr.dma("sp", GB[c * 128:(c + 1) * 128, :], gb[:], R=[gb], W=[GB])
        tr.barrier()


def toeplitz_conv(tr, NBo, srcs, Yc, pY, NCOL=None):
    W = (2 * NBo - 1) * 128
    NCOL = NCOL or NBo
    nper = 512 // NCOL
    strips = [[tr.sb([128, W], BF16, f"strip{si}_{i}") for i in range(3)] for si in range(len(srcs))]
    nmm = len(srcs) * (2 * NBo - 1)
    for c in range(256):
        bank = pY[(c // nper) % 2]
        k = 0
        for si, (G, Z) in enumerate(srcs):
            st = strips[si][c % 3]
            rowlen = G.t.shape[1]
            src = bass.AP(G.t.tensor, c * rowlen, [[1, 128], [1, W]])
            tr.dma("sp" if si == 0 else "act", st[:], src, R=[G], W=[st])
            for d in range(-(NBo - 1), NBo):
                tr.op("pe", lambda e: e.matmul(bank[:, (c % nper) * NCOL:(c % nper + 1) * NCOL], lhsT=st[:, (NBo - 1 - d) * 128:(NBo - d) * 128],
                                               rhs=Z[:, c, NBo - 1 - d:NBo - 1 - d + NCOL], start=(k == 0), stop=(k == nmm - 1)),
                      R=[st, Z], W=[bank], sig=(k == nmm - 1))
                k += 1
        if c % nper == nper - 1:
            c0 = c - nper + 1
            tr.op("dve", lambda e: e.tensor_copy(out=Yc[:, c0:c0 + nper, :], in_=bank[:, 0:512].rearrange("p (c i) -> p c i", i=NCOL)), R=[bank], W=[Yc])


def stage_hyena(nc, tr, projT, P, tabs, sel, GA, GB, GAc, YT, ident_bf, rev_f, need_ctx, stop_after=None):
    hyena_filter(nc, tr, P, tabs["ZF"], tabs["ZB"], tabs["DECF"], tabs["DECB"], 4096, 2047, GA, GB, sel)
    if need_ctx:
        hyena_filter(nc, tr, P, tabs["ZFc"], tabs["ZBc"], tabs["DECFc"], tabs["DECBc"], 256, 255, GAc, None, sel)
    NX = NTOK if need_ctx else NLAT
    NO = NQ if need_ctx else NOWN
    if stop_after == "filter":
        return
    with ExitStack() as es:
        tr.es = es
        col = lambda ap: ap.rearrange("(p o) -> p o", o=1)
        cw = tr.sb([128, 6, 3], F32, "cw"); cb = tr.sb([128, 6], F32, "cb")
        for cc in range(6):
            for j in range(3):
                tr.dma("sp", cw[:, cc, j:j + 1], col(P["conv_w"][j, cc * 128:(cc + 1) * 128]), R=[P["conv_w"]], W=[cw])
            tr.dma("sp", cb[:, cc:cc + 1], col(P["conv_b"][cc * 128:(cc + 1) * 128]), R=[P["conv_b"]], W=[cb])
        selt = tr.sb([128, 2], F32, "selt")
        tr.dma("sp", selt[:], sel[:], R=[sel], W=[selt])
        cwh = tr.sb([128, 6, 3], F32, "cwh"); ncwh = tr.sb([128, 6, 3], F32, "ncwh"); ncw = tr.sb([128, 6, 3], F32, "ncw")
        tr.op("dve", lambda e: e.tensor_scalar_mul(out=cwh[:], in0=cw[:], scalar1=selt[:, 1:2]), R=[cw, selt], W=[cwh])
        tr.op("dve", lambda e: e.tensor_scalar_mul(out=ncwh[:], in0=cwh[:], scalar1=-1.0), R=[cwh], W=[ncwh])
        tr.op("dve", lambda e: e.tensor_scalar_mul(out=ncw[:], in0=cw[:], scalar1=-1.0), R=[cw], W=[ncw])
        dsk = tr.sb([128, 2], F32, "dsk")
        for c in range(2):
            tr.dma("sp", dsk[:, c:c + 1], col(P["d_skip"][c * 128:(c + 1) * 128]), R=[P["d_skip"]], W=[dsk])
        zb = tr.sb([128, 2, NX], BF16, "zb")
        zf = tr.sb([128, 2, NO], F32, "zf")
        x0 = tr.sb([128, 2, NO], F32, "x0")
        Zown = tr.sb([128, 256, 46], BF16, "Zown")
        Zoth = tr.sb([128, 256, 46], BF16, "Zoth")
        tr.op("pool", lambda e: e.memset(Zown[:], 0.0), W=[Zown])
        tr.op("pool", lambda e: e.memset(Zoth[:], 0.0), W=[Zoth])
        if need_ctx:
            Zc = tr.sb([128, 256, 18], BF16, "Zc")
            tr.op("pool", lambda e: e.memset(Zc[:], 0.0), W=[Zc])
        with ExitStack() as es2:
            tr.es = es2
            U = tr.sb([128, NX], F32, "U")
            Cs = [tr.sb([128, NX], F32, f"C{i}") for i in range(2)]

            def fix(C, dcol, scol, wt, cc, j):
                tr.op("dve", lambda e: e.scalar_tensor_tensor(out=C[:, dcol:dcol + 1], in0=U[:, scol:scol + 1], scalar=wt[:, cc, j:j + 1], in1=C[:, dcol:dcol + 1], op0=ALU.mult, op1=ALU.add),
                      R=[U, wt, C], W=[C])

            def sconv(cc, C):
                tr.dma("sp", U[:], projT[768 + cc * 128:768 + (cc + 1) * 128, 0:NX], R=[projT], W=[U])
                tr.op("act", lambda e: e.activation(out=C[:], in_=U[:], func=AF.Identity, scale=cw[:, cc, 1:2], bias=cb[:, cc:cc + 1]), R=[U, cw, cb], W=[C])
                tr.op("dve", lambda e: e.scalar_tensor_tensor(out=C[:, 1:NX], in0=U[:, 0:NX - 1], scalar=cw[:, cc, 0:1], in1=C[:, 1:NX], op0=ALU.mult, op1=ALU.add), R=[U, cw, C], W=[C])
                tr.op("dve", lambda e: e.scalar_tensor_tensor(out=C[:, 0:NX - 1], in0=U[:, 1:NX], scalar=cw[:, cc, 2:3], in1=C[:, 0:NX - 1], op0=ALU.mult, op1=ALU.add), R=[U, cw, C], W=[C])
                fix(C, 2048, 2047, ncwh, cc, 0); fix(C, 2047, 2048, ncwh, cc, 2)
                fix(C, 0, 4095, cwh, cc, 0); fix(C, 4095, 0, cwh, cc, 2)
                if need_ctx:
                    fix(C, 4096, 4095, ncw, cc, 0); fix(C, 4095, 4096, ncw, cc, 2)

            for c in range(2):
                sconv(c, Cs[0])
                tr.op("pool", lambda e: e.tensor_copy(out=x0[:, c, 0:NOWN], in_=Cs[0][:, 0:NOWN]), R=[Cs[0]], W=[x0])
                if need_ctx:
                    tr.op("pool", lambda e: e.tensor_copy(out=x0[:, c, NOWN:NO], in_=Cs[0][:, NLAT:NX]), R=[Cs[0]], W=[x0])
                sconv(2 + c, Cs[0])
                sconv(4 + c, Cs[1])
                tr.op("dve", lambda e: e.tensor_mul(out=zb[:, c, :], in0=Cs[0][:], in1=Cs[1][:]), R=Cs, W=[zb])
                tr.op("pool", lambda e: e.tensor_mul(out=zf[:, c, 0:NOWN], in0=Cs[0][:, 0:NOWN], in1=Cs[1][:, 0:NOWN]), R=Cs, W=[zf])
                if need_ctx:
                    tr.op("pool", lambda e: e.tensor_mul(out=zf[:, c, NOWN:NO], in0=Cs[0][:, NLAT:NX], in1=Cs[1][:, NLAT:NX]), R=Cs, W=[zf])
            tr.barrier()
        tr.es = es
        if stop_after == "sconv":
            dbg = tr.sb([128, 512], BF16, "dbg")
            for c in range(2):
                tr.op("dve", lambda e: e.tensor_copy(out=dbg[:], in_=zf[:, c, 0:512]), R=[zf], W=[dbg])
                tr.dma("sp", YT[512 + c * 128:512 + (c + 1) * 128, 0:512], dbg[:], R=[dbg], W=[YT])
            tr.barrier()
            return
        pT = [tr.ps([128, 8, 128], BF16, f"hpT{i}") for i in range(2)]
        k = 0
        groups = [(Zown, 15, 0, 8), (Zown, 23, 8, 8), (Zoth, 15, 16, 8), (Zoth, 23, 24, 8)]
        if need_ctx:
            groups.append((Zc, 1, 32, 2))
        for (Zt, pad0, blk0, nb) in groups:
            for c in range(2):
                p = pT[k % 2]
                k += 1
                for j in range(nb):
                    tr.op("pe", lambda e: e.transpose(out=p[:, j, :], in_=zb[:, c, (blk0 + j) * 128:(blk0 + j + 1) * 128], identity=ident_bf[:]),
                          R=[zb, ident_bf], W=[p], sig=(j == nb - 1))
                tr.op("dve", lambda e: e.tensor_copy(out=Zt[:, c * 128:(c + 1) * 128, pad0:pad0 + nb].rearrange("p c j -> p j c"), in_=p[:, 0:nb, :]), R=[p], W=[Zt])
        if stop_after == "ztrans":
            tr.barrier()
            return
        pY = [tr.ps([128, 512], F32, f"pY{i}") for i in range(2)]
        pB = [tr.ps([128, 4, 128], F32, f"pBk{i}") for i in range(2)]
        tmp = [tr.sb([128, 512], F32, f"ytmp{i}") for i in range(2)]
        yo = [tr.sb([128, 512], BF16, f"yo{i}") for i in range(2)]

        def back(Yc, NBo, col0):
            kk = 0
            for c in range(2):
                for i0 in range(0, NBo, 4):
                    nb = min(4, NBo - i0)
                    p = pB[kk % 2]; t_ = tmp[kk % 2]; y_ = yo[kk % 2]
                    kk += 1
                    n = nb * 128
                    for j in range(nb):
                        tr.op("pe", lambda e: e.matmul(p[:, j, :], lhsT=Yc[:, c * 128:(c + 1) * 128, i0 + j], rhs=rev_f[:], start=True, stop=True),
                              R=[Yc, rev_f], W=[p], sig=(j == nb - 1))
                    cs = slice(col0 + i0 * 128, col0 + i0 * 128 + n)
                    tr.op("dve", lambda e: e.scalar_tensor_tensor(out=t_[:, 0:n], in0=zf[:, c, cs], scalar=dsk[:, c:c + 1], in1=p[:, 0:nb, :].rearrange("p j i -> p (j i)"), op0=ALU.mult, op1=ALU.add),
                          R=[zf, dsk, p], W=[t_])
                    tr.op("dve", lambda e: e.tensor_mul(out=y_[:, 0:n], in0=t_[:, 0:n], in1=x0[:, c, cs]), R=[t_, x0], W=[y_])
                    tr.dma("sp", YT[512 + c * 128:512 + (c + 1) * 128, cs], y_[:, 0:n], R=[y_], W=[YT])

        with ExitStack() as es3:
            tr.es = es3
            Yc = tr.sb([128, 256, 16], F32, "Yc")
            toeplitz_conv(tr, 16, [(GA, Zown), (GB, Zoth)], Yc, pY)
            if stop_after != "toep":
                back(Yc, 16, 0)
            tr.barrier()
        if need_ctx:
            with ExitStack() as es4:
                tr.es = es4
                Ycc = tr.sb([128, 256, 16], F32, "Ycc")
                toeplitz_conv(tr, 2, [(GAc, Zc)], Ycc, pY, NCOL=16)
                if stop_after != "toep":
                    back(Ycc, 2, NOWN)
                tr.barrier()
        tr.es = es
        tr.barrier()


ALPHA = (2.0 * 2) ** 0.25


def load_rep(tr, src_row_ap, srcbuf, n=1024, name="rep"):
    t = tr.sb([128, n], F32, name)
    tr.dma("sp", t[:], src_row_ap.partition_broadcast(128), R=[srcbuf], W=[t])
    return t


def ln_affine_store(tr, u, stats, mv, rstd, tmp, nmr, lng, lnb, dst, row0):
    ln_stats(tr, u, stats, mv, rstd, tmp)
    tr.op("dve", lambda e: e.scalar_tensor_tensor(out=nmr[:], in0=mv[:, 0:1], scalar=-1.0, in1=rstd[:], op0=ALU.mult, op1=ALU.mult), R=[mv, rstd], W=[nmr])
    tr.op("act", lambda e: e.activation(out=u[:], in_=u[:], func=AF.Identity, scale=rstd[:], bias=nmr[:]), R=[u, rstd, nmr], W=[u])
    tr.op("pool", lambda e: e.tensor_mul(out=u[:], in0=u[:], in1=lng[:]), R=[u, lng], W=[u])
    tr.op("dve", lambda e: e.tensor_add(out=u[:], in0=u[:], in1=lnb[:]), R=[u, lnb], W=[u])
    tr.dma("sp", dst[row0:row0 + 128, :], u[:], R=[u], W=[dst])


def stage_outproj(nc, tr, YT, w_out, xsrc, modrow, ln_g, ln_b, X1, need_ctx):
    NO = NQ if need_ctx else NOWN
    with ExitStack() as es:
        tr.es = es
        w = tr.sb([128, 8, D], BF16, "w_out")
        for k in range(8):
            tr.dma("pool", w[:, k, :], w_out[k * 128:(k + 1) * 128, :], R=[w_out], W=[w])
        g1 = [load_rep(tr, modrow[r, 2048:3072], modrow, name=f"g1_{r}") for r in range(2)]
        lng = load_rep(tr, ln_g[:], ln_g, name="lng"); lnb = load_rep(tr, ln_b[:], ln_b, name="lnb")
        yT = [tr.sb([128, 8, 512], BF16, f"yT{i}") for i in range(2)]
        xt = [tr.sb([128, D], F32, f"xo{i}") for i in range(2)]
        u = [tr.sb([128, D], F32, f"u{i}") for i in range(2)]
        po = [[tr.ps([128, 512], F32, f"po{i}{j}") for j in range(2)] for i in range(2)]
        lnb_ = [[tr.sb([128, 12], F32, f"stats{i}"), tr.sb([128, 2], F32, f"mv{i}"), tr.sb([128, 1], F32, f"rstd{i}"),
                 tr.sb([128, 1], F32, f"tmp{i}"), tr.sb([128, 1], F32, f"nmr{i}")] for i in range(2)]
        it = 0
        for b0 in range(0, NO, 512):
            nb = min(512, NO - b0)
            y = yT[(b0 // 512) % 2]
            for k in range(8):
                tr.dma("sp", y[:, k, 0:nb], YT[k * 128:(k + 1) * 128, b0:b0 + nb], R=[YT], W=[y])
            for ti in range(nb // 128):
                tok = b0 + ti * 128
                r = 1 if tok >= NOWN else 0
                srow = tok if r == 0 else NLAT + (tok - NOWN)
                x = xt[it % 2]; uu = u[it % 2]; pp = po[it % 2]
                stats, mv, rstd, tmp, nmr = lnb_[it % 2]
                it += 1
                tr.dma("sp", x[:], xsrc[srow:srow + 128, :], R=[xsrc], W=[x])
                for half in range(2):
                    for k in range(8):
                        tr.op("pe", lambda e: e.matmul(pp[half][:], lhsT=y[:, k, ti * 128:(ti + 1) * 128], rhs=w[:, k, half * 512:(half + 1) * 512],
                                                       start=(k == 0), stop=(k == 7)), R=[y, w], W=[pp[half]], sig=(k == 7))
                    tr.op("dve", lambda e: e.tensor_mul(out=uu[:, half * 512:(half + 1) * 512], in0=pp[half][:], in1=g1[r][:, half * 512:(half + 1) * 512]),
                          R=[pp[half], g1[r]], W=[uu])
                tr.op("dve", lambda e: e.scalar_tensor_tensor(out=uu[:], in0=x[:], scalar=ALPHA, in1=uu[:], op0=ALU.mult, op1=ALU.add), R=[x, uu], W=[uu])
                ln_affine_store(tr, uu, stats, mv, rstd, tmp, nmr, lng, lnb, X1, tok)
        tr.barrier()


def stage_moe(nc, tr, X1, modrow, router_w, router_b, exp_w1, exp_b1, exp_w2, exp_b2, ln_g, ln_b, XOUT, ident_f, need_ctx, NE=32):
    NO = NQ if need_ctx else NOWN
    NT = NO // 128
    with ExitStack() as es:
        tr.es = es
        h2T = tr.sb([128, 8, NO], BF16, "h2T")
        acc = [tr.sb([128, D], F32, f"acc{t}") for t in range(NT)]
        gates = tr.sb([128, NT, NE], F32, "gates")
        b1T = tr.sb([128, NE * 16], F32, "b1T")
        b1T1 = tr.sb([128, NE * 16], F32, "b1T1")
        with ExitStack() as es1:
            tr.es = es1
            mods = []
            for r in range(2):
                mT = load_modT(nc, tr, modrow, r, None)
                sp1 = tr.sb([128, 8], F32, "sp1")
                tr.op("dve", lambda e: e.tensor_scalar_add(out=sp1[:], in0=mT[:, 32:40], scalar1=1.0), R=[mT], W=[sp1])
                mods.append((mT, sp1))
            rw = tr.sb([128, 8, NE], F32, "rw")
            tr.dma("sp", rw[:], router_w[:, :].rearrange("(k p) n -> p k n", p=128), R=[router_w], W=[rw])
            rb = tr.sb([128, NE], F32, "rb")
            tr.dma("sp", rb[:], router_b[:].partition_broadcast(128), R=[router_b], W=[rb])
            b2 = tr.sb([NE, D], F32, "b2")
            tr.dma("sp", b2[:], exp_b2[:, :], R=[exp_b2], W=[b2])
            GT = tr.sb([NE, NO], F32, "GT")
            pT = [tr.ps([128, 4, 128], F32, f"mpT{i}") for i in range(2)]
            pR = tr.ps([128, 512], F32, "pR")
            pG = tr.ps([128, 512], F32, "pGt")
            pA = [tr.ps([128, 512], F32, f"pA{i}") for i in range(2)]
            nblk = NE * 16 // 128
            b1raw = tr.sb([128, nblk, 128], F32, "b1raw")
            tr.dma("sp", b1raw[:], exp_b1[:, :].rearrange("e (c p) -> (e c) p", p=128).rearrange("(b r) p -> r b p", r=128), R=[exp_b1], W=[b1raw])
            for bk in range(nblk):
                tr.op("pe", lambda e: e.transpose(out=pR[:, 0:128], in_=b1raw[:, bk, :], identity=ident_f[:]), R=[b1raw, ident_f], W=[pR])
                tr.op("dve", lambda e: e.tensor_copy(out=b1T[:, bk * 128:(bk + 1) * 128], in_=pR[:, 0:128]), R=[pR], W=[b1T])
            tr.op("dve", lambda e: e.tensor_scalar_add(out=b1T1[:], in0=b1T[:], scalar1=1.0), R=[b1T], W=[b1T1])
            xt = [tr.sb([128, D], F32, f"mx{i}") for i in range(2)]
            xn = [tr.sb([128, D], F32, f"mxn{i}") for i in range(2)]
            hf = [tr.sb([128, 8, 128], F32, f"hf{i}") for i in range(2)]
            lnb_ = [[tr.sb([128, 12], F32, f"stats{i}"), tr.sb([128, 2], F32, f"mv{i}"), tr.sb([128, 1], F32, f"rstd{i}"),
                     tr.sb([128, 1], F32, f"tmp{i}"), tr.sb([128, 1], F32, f"nmr{i}")] for i in range(2)]
            rtb_ = [[tr.sb([128, NE], F32, f"lg{i}"), tr.sb([128, 8], F32, f"m8{i}"), tr.sb([128, 1], F32, f"nmx{i}"), tr.sb([128, NE], F32, f"msk{i}"),
                     tr.sb([128, NE], F32, f"ex{i}"), tr.sb([128, 1], F32, f"ssum{i}"), tr.sb([128, 1], F32, f"rs{i}")] for i in range(2)]
            for t in range(NT):
                tok = t * 128
                stats, mv, rstd, tmp, nmr = lnb_[t % 2]
                lg, m8, nmx, msk, ex, ssum, rs = rtb_[t % 2]
                r = 1 if tok >= NOWN else 0
                mT, sp1 = mods[r]
                x = xt[t % 2]; xx = xn[t % 2]; h = hf[t % 2]
                tr.dma("sp", x[:], X1[tok:tok + 128, :], R=[X1], W=[x])
                ln_stats(tr, x, stats, mv, rstd, tmp)
                tr.op("dve", lambda e: e.scalar_tensor_tensor(out=nmr[:], in0=mv[:, 0:1], scalar=-1.0, in1=rstd[:], op0=ALU.mult, op1=ALU.mult), R=[mv, rstd], W=[nmr])
                tr.op("act", lambda e: e.activation(out=xx[:], in_=x[:], func=AF.Identity, scale=rstd[:], bias=nmr[:]), R=[x, rstd, nmr], W=[xx])
                for c in range(8):
                    p = pT[(c // 4) % 2]
                    tr.op("pe", lambda e: e.transpose(out=p[:, c % 4, :], in_=xx[:, c * 128:(c + 1) * 128], identity=ident_f[:]), R=[xx, ident_f], W=[p])
                    tr.op("dve" if c % 2 else "act",
                          (lambda e: e.tensor_scalar(out=h[:, c, :], in0=p[:, c % 4, :], scalar1=sp1[:, c:c + 1], scalar2=mT[:, 24 + c:25 + c], op0=ALU.mult, op1=ALU.add)) if c % 2 else
                          (lambda e: e.activation(out=h[:, c, :], in_=p[:, c % 4, :], func=AF.Identity, scale=sp1[:, c:c + 1], bias=mT[:, 24 + c:25 + c])),
                          R=[p, sp1, mT], W=[h])
                tr.op("pool", lambda e: e.tensor_copy(out=h2T[:, :, tok:tok + 128], in_=h[:]), R=[h], W=[h2T])
                for c in range(8):
                    tr.op("pe", lambda e: e.matmul(pR[:, 0:NE], lhsT=h[:, c, :], rhs=rw[:, c, :], start=(c == 0), stop=(c == 7)), R=[h, rw], W=[pR], sig=(c == 7))
                tr.op("dve", lambda e: e.tensor_add(out=lg[:], in0=pR[:, 0:NE], in1=rb[:]), R=[pR, rb], W=[lg])
                tr.op("dve", lambda e: e.max(out=m8[:], in_=lg[:]), R=[lg], W=[m8])
                tr.op("dve", lambda e: e.tensor_scalar(out=msk[:], in0=lg[:], scalar1=m8[:, 3:4], scalar2=None, op0=ALU.is_ge), R=[lg, m8], W=[msk])
                tr.op("dve", lambda e: e.tensor_scalar_mul(out=nmx[:], in0=m8[:, 0:1], scalar1=-1.0), R=[m8], W=[nmx])
                tr.op("act", lambda e: e.activation(out=ex[:], in_=lg[:], func=AF.Exp, bias=nmx[:]), R=[lg, nmx], W=[ex])
                tr.op("dve", lambda e: e.tensor_mul(out=ex[:], in0=ex[:], in1=msk[:]), R=[ex, msk], W=[ex])
                tr.op("dve", lambda e: e.reduce_sum(out=ssum[:], in_=ex[:], axis=AX.X), R=[ex], W=[ssum])
                tr.op("dve", lambda e: e.reciprocal(out=rs[:], in_=ssum[:]), R=[ssum], W=[rs])
                tr.op("dve", lambda e: e.tensor_scalar_mul(out=gates[:, t, :], in0=ex[:], scalar1=rs[:, 0:1]), R=[ex, rs], W=[gates])
                tr.op("pe", lambda e: e.transpose(out=pG[0:NE, 0:128], in_=gates[:, t, :], identity=ident_f[:]), R=[gates, ident_f], W=[pG])
                tr.op("dve", lambda e: e.tensor_copy(out=GT[:, tok:tok + 128], in_=pG[0:NE, 0:128]), R=[pG], W=[GT])
                for half in range(2):
                    pa = pA[half]
                    tr.op("pe", lambda e: e.matmul(pa[:], lhsT=GT[:, tok:tok + 128], rhs=b2[:, half * 512:(half + 1) * 512], start=True, stop=True), R=[GT, b2], W=[pa])
                    tr.op("act", lambda e: e.activation(out=acc[t][:, half * 512:(half + 1) * 512], in_=pa[:], func=AF.Copy), R=[pa], W=[acc[t]])
            tr.barrier()
        tr.es = es
        with ExitStack() as es2:
            tr.es = es2
            w1h = [tr.sb([128, 8, 1024], BF16, f"w1h{i}") for i in range(2)]
            w2h = [tr.sb([128, 4, 1024], BF16, f"w2h{i}") for i in range(2)]
            actT = [tr.sb([128, 4, 512], BF16, f"actT{i}") for i in range(2)]
            gsb = [tr.sb([128, 512], BF16, f"g{i}") for i in range(2)]
            sig = [tr.sb([128, 512], BF16, f"sg{i}") for i in range(2)]
            gs = [tr.sb([128, 512], BF16, f"gs{i}") for i in range(2)]
            tl = [tr.sb([128, 512], F32, f"tl{i}") for i in range(2)]
            pGm = [tr.ps([128, 512], F32, f"pGm{i}") for i in range(2)]
            pLm = [tr.ps([128, 512], F32, f"pLm{i}") for i in range(2)]
            pO = [[tr.ps([128, 512], F32, f"pOm{i}{j}") for j in range(2)] for i in range(2)]
            ui = 0
            ei = 0
            oi = 0

            def load_unit(u):
                e_, s = u // 2, u % 2
                w1 = w1h[u % 2]; w2 = w2h[u % 2]
                tr.dma("pool", w1[:, :, 0:512], exp_w1[e_, :, s * 512:(s + 1) * 512].rearrange("(k p) n -> p k n", p=128), R=[exp_w1], W=[w1])
                tr.dma("pool", w1[:, :, 512:1024], exp_w1[e_, :, 1024 + s * 512:1024 + (s + 1) * 512].rearrange("(k p) n -> p k n", p=128), R=[exp_w1], W=[w1])
                tr.dma("pool", w2[:], exp_w2[e_, s * 512:(s + 1) * 512, :].rearrange("(k p) n -> p k n", p=128), R=[exp_w2], W=[w2])

            blocks = [(b0, min(512, NO - b0)) for b0 in range(0, NO, 512)]
            items = [(u, bi) for u in range(2 * NE) for bi in range(len(blocks))]
            state = {"ei": 0, "oi": 0}

            def w1_phase(i):
                u, bi = items[i]
                e_, s = u // 2, u % 2
                b0, nb = blocks[bi]
                w1 = w1h[u % 2]
                aT = actT[i % 2]
                for m in range(4):
                    j = state["ei"] % 2
                    state["ei"] += 1
                    cg = e_ * 16 + s * 4 + m
                    cl = e_ * 16 + 8 + s * 4 + m
                    for k in range(8):
                        tr.op("pe", lambda e: e.matmul(pGm[j][:, 0:nb], lhsT=w1[:, k, m * 128:(m + 1) * 128], rhs=h2T[:, k, b0:b0 + nb], start=(k == 0), stop=(k == 7)),
                              R=[w1, h2T], W=[pGm[j]], sig=(k == 7))
                    for k in range(8):
                        tr.op("pe", lambda e: e.matmul(pLm[j][:, 0:nb], lhsT=w1[:, k, 512 + m * 128:512 + (m + 1) * 128], rhs=h2T[:, k, b0:b0 + nb], start=(k == 0), stop=(k == 7)),
                              R=[w1, h2T], W=[pLm[j]], sig=(k == 7))
                    tr.op("dve", lambda e: e.tensor_scalar(out=gsb[j][:, 0:nb], in0=pGm[j][:, 0:nb], scalar1=b1T[:, cg:cg + 1], scalar2=7.0, op0=ALU.add, op1=ALU.min),
                          R=[pGm[j], b1T], W=[gsb[j]])
                    tr.op("act", lambda e: e.activation(out=sig[j][:, 0:nb], in_=gsb[j][:, 0:nb], func=AF.Sigmoid, scale=1.702), R=[gsb[j]], W=[sig[j]])
                    tr.op("pool", lambda e: e.tensor_mul(out=gs[j][:, 0:nb], in0=gsb[j][:, 0:nb], in1=sig[j][:, 0:nb]), R=[gsb[j], sig[j]], W=[gs[j]])
                    tr.op("dve", lambda e: e.tensor_scalar(out=tl[j][:, 0:nb], in0=pLm[j][:, 0:nb], scalar1=b1T1[:, cl:cl + 1], scalar2=-6.0, op0=ALU.add, op1=ALU.max),
                          R=[pLm[j], b1T1], W=[tl[j]])
                    tr.op("dve", lambda e: e.scalar_tensor_tensor(out=aT[:, m, 0:nb], in0=tl[j][:, 0:nb], scalar=8.0, in1=gs[j][:, 0:nb], op0=ALU.min, op1=ALU.mult),
                          R=[tl[j], gs[j]], W=[aT])

            def w2_phase(i):
                u, bi = items[i]
                e_ = u // 2
                b0, nb = blocks[bi]
                w2 = w2h[u % 2]
                aT = actT[i % 2]
                for ti in range(nb // 128):
                    t = (b0 // 128) + ti
                    pp = pO[state["oi"] % 2]
                    state["oi"] += 1
                    for half in range(2):
                        for m in range(4):
                            tr.op("pe", lambda e: e.matmul(pp[half][:], lhsT=aT[:, m, ti * 128:(ti + 1) * 128], rhs=w2[:, m, half * 512:(half + 1) * 512], start=(m == 0), stop=(m == 3)),
                                  R=[aT, w2], W=[pp[half]], sig=(m == 3))
                        tr.op("dve", lambda e: e.scalar_tensor_tensor(out=acc[t][:, half * 512:(half + 1) * 512], in0=pp[half][:], scalar=gates[:, t, e_:e_ + 1],
                                                                      in1=acc[t][:, half * 512:(half + 1) * 512], op0=ALU.mult, op1=ALU.add),
                              R=[pp[half], gates, acc[t]], W=[acc[t]])

            load_unit(0)
            if 2 * NE > 1:
                load_unit(1)
            w1_phase(0)
            for i in range(len(items)):
                u, bi = items[i]
                if i + 1 < len(items):
                    w1_phase(i + 1)
                w2_phase(i)
                if bi == len(blocks) - 1 and u + 2 < 2 * NE:
                    load_unit(u + 2)
            tr.barrier()
        tr.es = es
        with ExitStack() as es3:
            tr.es = es3
            g2 = [load_rep(tr, modrow[r, 5120:6144], modrow, name=f"g2_{r}") for r in range(2)]
            lng = load_rep(tr, ln_g[:], ln_g, name="lng2"); lnb = load_rep(tr, ln_b[:], ln_b, name="lnb2")
            xt = [tr.sb([128, D], F32, f"fx{i}") for i in range(2)]
            u = [tr.sb([128, D], F32, f"fu{i}") for i in range(2)]
            lnb_ = [[tr.sb([128, 12], F32, f"stats{i}"), tr.sb([128, 2], F32, f"mv{i}"), tr.sb([128, 1], F32, f"rstd{i}"),
                     tr.sb([128, 1], F32, f"tmp{i}"), tr.sb([128, 1], F32, f"nmr{i}")] for i in range(2)]
            for t in range(NT):
                tok = t * 128
                r = 1 if tok >= NOWN else 0
                x = xt[t % 2]; uu = u[t % 2]
                stats, mv, rstd, tmp, nmr = lnb_[t % 2]
                tr.dma("sp", x[:], X1[tok:tok + 128, :], R=[X1], W=[x])
                tr.op("pool", lambda e: e.tensor_mul(out=uu[:], in0=acc[t][:], in1=g2[r][:]), R=[acc[t], g2[r]], W=[uu])
                tr.op("dve", lambda e: e.scalar_tensor_tensor(out=uu[:], in0=x[:], scalar=ALPHA, in1=uu[:], op0=ALU.mult, op1=ALU.add), R=[x, uu], W=[uu])
                ln_affine_store(tr, uu, stats, mv, rstd, tmp, nmr, lng, lnb, XOUT, tok)
            tr.barrier()
        tr.es = es
        tr.barrier()


GRID_W = 64


def local_token_ids(h):
    own = np.arange(2048) + 2048 * h
    oth = np.arange(2048) + 2048 * (1 - h)
    return np.concatenate([own, oth])


def rope_tables(h):
    t = local_token_ids(h)
    row = (t // GRID_W).astype(np.float32)[:, None]
    col = (t % GRID_W).astype(np.float32)[:, None]
    inv_freq = (np.float32(10000.0) ** (-np.arange(0, 32, 2, dtype=np.float32) / np.float32(32))).astype(np.float32)
    ang_r = row * inv_freq
    ang_c = col * inv_freq
    ang = np.concatenate([ang_r, ang_r, ang_c, ang_c], axis=-1)
    cos = np.cos(ang).astype(np.float32)
    sin = np.sin(ang).astype(np.float32)
    d = np.arange(64)
    sign = np.where((d % 32) < 16, -1.0, 1.0).astype(np.float32)
    sins = sin * sign
    cos = np.concatenate([cos, np.ones((256, 64), np.float32)], 0)
    sins = np.concatenate([sins, np.zeros((256, 64), np.float32)], 0)
    cosT = np.ascontiguousarray(np.concatenate([cos.T, cos.T], 0))
    sinT = np.ascontiguousarray(np.concatenate([sins.T, sins.T], 0))
    return cosT, sinT


def perm_blockones():
    import ml_dtypes
    d = np.arange(128)
    sw = np.where((d % 32) < 16, d + 16, d - 16)
    perm = np.zeros((128, 128), np.float32)
    perm[sw, d] = 1.0
    bo = (d[:, None] // 64 == d[None, :] // 64).astype(np.float32)
    return perm.astype(ml_dtypes.bfloat16), bo.astype(ml_dtypes.bfloat16)


def na_bias_tables(rpb, h):
    out = np.full((4, 33, 128, 128), -30000.0, np.float32)
    kk = np.arange(128)
    qq = np.arange(128)
    entries = []
    for s in range(1, 6):
        entries.append((s - 1, 16 * h + 2, s))
    for spec, u in enumerate([0, 1, 14, 15]):
        for s in range(7):
            entries.append((5 + spec * 7 + s, 16 * h + u, s))
    for idx, t, s in entries:
        j = t + s - 3
        kr = (2 * j + kk // 64)[:, None]
        kc = (kk % 64)[:, None]
        r = (2 * t + qq // 64)[None, :]
        qc = (qq % 64)[None, :]
        rs = np.clip(r - 4, 0, 56)
        cs = np.clip(qc - 8, 0, 48)
        valid = (kr >= 0) & (kr <= 63) & (kr >= rs) & (kr <= rs + 7) & (kc >= cs) & (kc <= cs + 15)
        rr = np.clip(kr - r + 7, 0, 14)
        rc = np.clip(kc - qc + 15, 0, 30)
        for hd in range(4):
            g = rpb[hd][rr, rc]
            out[hd, idx] = np.where(valid, g, np.float32(-30000.0))
    return out


def hyena_tables(L):
    import math
    f32 = np.float32
    HY_EMB = 33; HY_BANDS = 16; HY_W = 256
    mn = math.log(1e-2) / 1.5; mx = math.log(1e-2) / 0.3
    t = np.linspace(0.0, 1.0, L, dtype=f32)[:, None]
    w = (f32(2.0 * math.pi / L) * np.arange(L, dtype=f32))[:, None]
    bands = np.linspace(1e-4, HY_BANDS - 1, HY_BANDS, dtype=f32)[None, :]
    z = np.concatenate([t, np.cos(bands * w), -np.sin(bands * w)], axis=-1).astype(f32)
    deltas = np.linspace(mn, mx, HY_W, dtype=f32)
    dec = np.exp(-t * np.abs(deltas)[None, :]).astype(f32)
    ZF = np.ascontiguousarray(z[::-1].T)
    ZB = np.ascontiguousarray(z.T)
    DECF = np.ascontiguousarray(dec[::-1].T)
    DECB = np.ascontiguousarray(dec.T)
    DECB[:, 0] = 0.0
    return ZF, ZB, DECF, DECB


_PROG_CACHE = {}

_IN_SPECS = [
    ("xsrc", [NTOK, D], F32), ("c2T", [128, 8, 2], F32), ("ada_w", [D, 6144], F32), ("ada_b", [6144], F32),
    ("w_in", [D, DIN], F32), ("w_out", [D, D], F32), ("q_gain", [64], F32), ("k_gain", [64], F32),
    ("conv_w", [3, 768], F32), ("conv_b", [768], F32), ("f_w1", [33, 64], F32), ("f_b1", [64], F32), ("f_freq", [2, 64], F32),
    ("f_w2", [64, 64], F32), ("f_b2", [64], F32), ("f_w3", [64, 512], F32), ("f_b3", [512], F32), ("d_skip", [256], F32),
    ("nb_bias", [4, 33, 128, 128], F32), ("ln1_g", [D], F32), ("ln1_b", [D], F32), ("ln2_g", [D], F32), ("ln2_b", [D], F32),
    ("router_w", [D, 32], F32), ("router_b", [32], F32), ("exp_w1", [32, D, 2048], F32), ("exp_b1", [32, 2048], F32),
    ("exp_w2", [32, D, D], F32), ("exp_b2", [32, D], F32),
    ("costab", [128, NTOK], F32), ("sintab", [128, NTOK], F32), ("blockones", [128, 128], BF16), ("perm", [128, 128], BF16),
    ("ZF", [33, 4096], F32), ("ZB", [33, 4096], F32), ("DECF", [256, 4096], F32), ("DECB", [256, 4096], F32),
    ("ZFc", [33, 256], F32), ("ZBc", [33, 256], F32), ("DECFc", [256, 256], F32), ("DECBc", [256, 256], F32),
    ("sel", [128, 2], F32), ("identb", [128, 128], BF16), ("identf", [128, 128], F32), ("revf", [128, 128], F32),
]


def build_layer(need_ctx):
    nc = bass.Bass("TRN2", target_bir_lowering=False)
    NO = NQ if need_ctx else NOWN
    with ExitStack() as es0:
        tr = TR(nc, es0)
        A = {}
        for name, shape, dt in _IN_SPECS:
            A[name] = Buf(nc.dram_tensor(name, shape, dt, kind="ExternalInput").ap(), name)
        XOUT = tr.dram("xout", [NO, D], F32, kind="ExternalOutput")
        modrow = tr.dram("modrow", [2, 6144], F32)
        projT = tr.dram("projT", [DIN, NTOK], F32)
        vtm = tr.dram("vtm", [NTOK, 384], BF16)
        QT = tr.dram("QT", [512, NTOK], BF16)
        KT = tr.dram("KT", [128, NTOK], BF16)
        YT = tr.dram("YT", [1024, NQ], BF16)
        GA = tr.dram("GA", [256, 4096], BF16)
        GB = tr.dram("GB", [256, 4096], BF16)
        GAc = tr.dram("GAc", [256, 512], BF16)
        X1 = tr.dram("X1", [NQ, D], F32)
        with ExitStack() as esp:
            tr.es = esp
            ident_bf = tr.sb([128, 128], BF16, "identb")
            ident_f = tr.sb([128, 128], F32, "identf")
            rev_f = tr.sb([128, 128], F32, "revf")
            tr.dma("sp", ident_bf[:], A["identb"][:], W=[ident_bf])
            tr.dma("sp", ident_f[:], A["identf"][:], W=[ident_f])
            tr.dma("sp", rev_f[:], A["revf"][:], W=[rev_f])
            stage_mod(nc, tr, A["c2T"], A["ada_w"], A["ada_b"], modrow)
            stage_inproj(nc, tr, A["xsrc"], modrow, A["w_in"], projT, vtm, ident_bf)
            stage_qkprep(nc, tr, projT, A["q_gain"], A["k_gain"], A["costab"], A["sintab"], A["blockones"], A["perm"], QT, KT, need_ctx)
            stage_gqa(nc, tr, QT, KT, vtm, YT, need_ctx)
            stage_na(nc, tr, projT, vtm, A["nb_bias"], YT, need_ctx)
            P = {k: A[k] for k in ("conv_w", "conv_b", "f_w1", "f_b1", "f_freq", "f_w2", "f_b2", "f_w3", "f_b3", "d_skip")}
            stage_hyena(nc, tr, projT, P, A, A["sel"], GA, GB, GAc, YT, ident_bf, rev_f, need_ctx)
            stage_outproj(nc, tr, YT, A["w_out"], A["xsrc"], modrow, A["ln1_g"], A["ln1_b"], X1, need_ctx)
            stage_moe(nc, tr, X1, modrow, A["router_w"], A["router_b"], A["exp_w1"], A["exp_b1"], A["exp_w2"], A["exp_b2"],
                      A["ln2_g"], A["ln2_b"], XOUT, ident_f, need_ctx)
            tr.finish()
        mx = max(tr.cnt.values())
        assert mx < 60000, f"semaphore count too large: {mx}"
    return nc


def _const_tables(h):
    import ml_dtypes
    cosT, sinT = rope_tables(h)
    perm, bo = perm_blockones()
    ZF, ZB, DECF, DECB = hyena_tables(4096)
    ZFc, ZBc, DECFc, DECBc = hyena_tables(256)
    eye = np.eye(128, dtype=np.float32)
    return dict(costab=cosT, sintab=sinT, blockones=bo, perm=perm, ZF=ZF, ZB=ZB, DECF=DECF, DECB=DECB,
                ZFc=ZFc, ZBc=ZBc, DECFc=DECFc, DECBc=DECBc,
                sel=np.tile(np.array([[1 - h, h]], np.float32), (128, 1)),
                identb=eye.astype(ml_dtypes.bfloat16), identf=eye, revf=np.ascontiguousarray(eye[::-1]))


def kernel(x, c, ctx, c_ctx, ada_w, ada_b, w_in, w_out, q_gain, k_gain, hy_conv_w, hy_conv_b,
           hy_w1, hy_b1, hy_freq, hy_w2, hy_b2, hy_w3, hy_b3, hy_d, na_rpb, ln1_g, ln1_b,
           ln2_g, ln2_b, router_w, router_b, exp_w1, exp_b1, exp_w2, exp_b2):
    f = lambda a: np.ascontiguousarray(np.asarray(a, dtype=np.float32))
    x = f(x); ctx = f(ctx); c = f(c); c_ctx = f(c_ctx)
    B = x.shape[0]
    consts = [_const_tables(h) for h in range(2)]
    for l in range(2):
        need_ctx = (l == 0)
        if need_ctx not in _PROG_CACHE:
            _PROG_CACHE[need_ctx] = build_layer(need_ctx)
        nc = _PROG_CACHE[need_ctx]
        shared = dict(ada_w=f(ada_w[l]), ada_b=f(ada_b[l]), w_in=f(w_in[l]), w_out=f(w_out[l]), q_gain=f(q_gain[l]), k_gain=f(k_gain[l]),
                      conv_w=f(hy_conv_w[l]), conv_b=f(hy_conv_b[l]), f_w1=f(hy_w1[l]), f_b1=f(hy_b1[l]), f_freq=f(hy_freq[l]),
                      f_w2=f(hy_w2[l]), f_b2=f(hy_b2[l]), f_w3=f(hy_w3[l]), f_b3=f(hy_b3[l]), d_skip=f(hy_d[l]),
                      ln1_g=f(ln1_g[l]), ln1_b=f(ln1_b[l]), ln2_g=f(ln2_g[l]), ln2_b=f(ln2_b[l]),
                      router_w=f(router_w[l]), router_b=f(router_b[l]), exp_w1=f(exp_w1[l]), exp_b1=f(exp_b1[l]),
                      exp_w2=f(exp_w2[l]), exp_b2=f(exp_b2[l]))
        nbb = [na_bias_tables(f(na_rpb[l]), h) for h in range(2)]
        in_maps = []
        for k in range(8):
            b, h = k // 2, k % 2
            ids = local_token_ids(h)
            xs = np.concatenate([x[b][ids], ctx[b]], axis=0)
            c2 = np.stack([c[b], c_ctx], 0)
            c2T = np.ascontiguousarray(c2.reshape(2, 8, 128).transpose(2, 1, 0))
            m = dict(xsrc=np.ascontiguousarray(xs), c2T=c2T, nb_bias=nbb[h])
            m.update(shared)
            m.update(consts[h])
            in_maps.append(m)
        res = run_bass_kernel_spmd(nc, in_maps, core_ids=list(range(8)))
        xn = np.empty_like(x)
        cn = np.empty_like(ctx)
        for k in range(8):
            b, h = k // 2, k % 2
            o = np.asarray(res.results[k]["xout"])
            xn[b, 2048 * h:2048 * (h + 1)] = o[:NOWN]
            if need_ctx and h == 0:
                cn[b] = o[NOWN:]
        x = xn
        if need_ctx:
            ctx = cn
    return x
```
